# Optimizing a Trainium2 kernel written in Bass

```python
import jax, jax.numpy as jnp
from jax import lax
import numpy as np

D_MODEL = 1024
BATCH = 16
SEQ = 2048
DEPTH = 1

GRID_W = 64
CTX_LEN = 256
D_MIX = D_MODEL
HEAD_DIM = 64
D_NA = D_MIX // 2
D_RW = D_MIX - D_NA
H_NA = D_NA // HEAD_DIM
H_RW = D_RW // HEAD_DIM
NA_KH = 8
NA_KW = 16
NA_CB = 16
NA_CBW = 2 * NA_CB
RW_LORA_W = 64
RW_LORA_A = 64
RW_LORA_G = 128
N_DIR = 2
D_RW_IN = 3 * D_RW + N_DIR * (RW_LORA_W + RW_LORA_A) + RW_LORA_G
D_IN = 3 * D_NA + D_RW_IN
MOE_GROUPS = 4
MOE_PER_GROUP = 8
N_EXPERTS = MOE_GROUPS * MOE_PER_GROUP
MOE_TOPK = 2
D_EXPERT = 512
ROPE_THETA = 10000.0
NORM_EPS = 1e-6
RW_LN_EPS = 64e-5
NEG = -1e30

kernel_name = 'hymba_natten_rwkv7_hmoe_dit_block'


def rmsnorm(x, g):
    xf = x.astype(jnp.float32)
    y = xf * lax.rsqrt(jnp.mean(xf * xf, axis=-1, keepdims=True) + NORM_EPS)
    return (y * g.astype(jnp.float32)).astype(x.dtype)


def modulate(h, shift, scale):
    return h * (1 + scale) + shift


def axial_rope(t):
    B, T, H, hd = t.shape
    nf = hd // 4
    pos = jnp.arange(T)
    inv = ROPE_THETA ** (-jnp.arange(nf, dtype=jnp.float32) / nf)
    ang = jnp.stack([(pos // GRID_W).astype(jnp.float32)[:, None] * inv,
                     (pos % GRID_W).astype(jnp.float32)[:, None] * inv], axis=1)
    cos = jnp.cos(ang)[None, :, None].astype(t.dtype)
    sin = jnp.sin(ang)[None, :, None].astype(t.dtype)
    t = t.reshape(B, T, H, 2, 2, nf)
    t1, t2 = t[..., 0, :], t[..., 1, :]
    return jnp.stack([t1 * cos - t2 * sin, t2 * cos + t1 * sin], axis=-2).reshape(B, T, H, hd)


def na_heads(p, q_g, k_g):
    B, T, _ = p.shape
    q, k, v = jnp.split(p, 3, axis=-1)
    sh = lambda t: t.reshape(B, T, H_NA, HEAD_DIM)
    q = rmsnorm(sh(q), q_g).transpose(0, 2, 1, 3)
    k = rmsnorm(sh(k), k_g).transpose(0, 2, 1, 3)
    return q, k, sh(v).transpose(0, 2, 1, 3)


def na_latent(q, k, v, kc, vc, rpb):
    B, H, T, hd = q.shape
    rows = T // GRID_W
    kh = min(NA_KH, rows)
    qb = 8 if rows % 8 == 0 else (4 if rows % 4 == 0 else 2)
    bh = min(kh + qb - 1, rows)
    n_rb, n_cb = rows // qb, GRID_W // NA_CB
    row_start = jnp.clip(jnp.arange(rows) - kh // 2, 0, rows - kh)
    col_start = jnp.clip(jnp.arange(GRID_W) - NA_KW // 2, 0, GRID_W - NA_KW)
    band_r = jnp.clip(row_start[::qb], 0, rows - bh)[:, None] + jnp.arange(bh)
    band_c = jnp.clip(col_start[::NA_CB], 0, GRID_W - NA_CBW)[:, None] + jnp.arange(NA_CBW)

    def band(t):
        g = t.reshape(B, H, rows, GRID_W, hd)
        g = jnp.take(g, band_r, axis=2)
        g = jnp.take(g, band_c, axis=4)
        return g.transpose(0, 1, 2, 4, 3, 5, 6).reshape(B, H, n_rb, n_cb, bh * NA_CBW, hd)

    kb, vb = band(k), band(v)
    qg = q.reshape(B, H, n_rb, qb, n_cb, NA_CB, hd).transpose(0, 1, 2, 4, 3, 5, 6)
    qg = qg.reshape(B, H, n_rb, n_cb, qb * NA_CB, hd)

    q_row = jnp.arange(n_rb)[:, None] * qb + jnp.arange(qb)
    q_col = jnp.arange(n_cb)[:, None] * NA_CB + jnp.arange(NA_CB)
    rs = row_start[q_row][:, :, None]
    row_ok = (band_r[:, None, :] >= rs) & (band_r[:, None, :] < rs + kh)
    cs = col_start[q_col][:, :, None]
    col_ok = (band_c[:, None, :] >= cs) & (band_c[:, None, :] < cs + NA_KW)
    ok = (row_ok[:, None, :, None, :, None] & col_ok[None, :, None, :, None, :])
    ok = ok.reshape(n_rb, n_cb, qb * NA_CB, bh * NA_CBW)
    dr = jnp.clip(band_r[:, None, :] - q_row[:, :, None], -(NA_KH - 1), NA_KH - 1) + NA_KH - 1
    dc = jnp.clip(band_c[:, None, :] - q_col[:, :, None], -(NA_KW - 1), NA_KW - 1) + NA_KW - 1
    bias = rpb[:, dr[:, None, :, None, :, None], dc[None, :, None, :, None, :]]
    bias = bias.reshape(H, n_rb, n_cb, qb * NA_CB, bh * NA_CBW).astype(jnp.float32)

    scale = hd ** -0.5
    s_win = jnp.einsum('bhrcqd,bhrckd->bhrcqk', qg, kb).astype(jnp.float32) * scale + bias[None]
    s_win = jnp.where(ok, s_win, NEG)
    s_ctx = jnp.einsum('bhrcqd,bhkd->bhrcqk', qg, kc).astype(jnp.float32) * scale
    p = jax.nn.softmax(jnp.concatenate([s_win, s_ctx], axis=-1), axis=-1).astype(v.dtype)
    n_win = bh * NA_CBW
    o = (jnp.einsum('bhrcqk,bhrckd->bhrcqd', p[..., :n_win], vb)
         + jnp.einsum('bhrcqk,bhkd->bhrcqd', p[..., n_win:], vc))
    o = o.reshape(B, H, n_rb, n_cb, qb, NA_CB, hd).transpose(0, 2, 4, 3, 5, 1, 6)
    return o.reshape(B, T, H * hd)


def na_context(qc, kc, vc):
    B, H, T, hd = qc.shape
    s = jnp.einsum('bhqd,bhkd->bhqk', qc, kc).astype(jnp.float32) * hd ** -0.5
    p = jax.nn.softmax(s, axis=-1).astype(vc.dtype)
    return jnp.einsum('bhqk,bhkd->bqhd', p, vc).reshape(B, T, H * hd)


def centred_shift(p, mu_prev, mu_next):
    prev = jnp.pad(p, ((0, 0), (1, 0), (0, 0)))[:, :-1]
    nxt = jnp.pad(p, ((0, 0), (0, 1), (0, 0)))[:, 1:]
    return p + mu_prev * (prev - p) + mu_next * (nxt - p)


def rwkv_streams(p, mu_prev, mu_next, w0, w_up, a0, a_up, g_up, k_k, k_a, rope):
    B, T, _ = p.shape
    p = centred_shift(p, mu_prev, mu_next)
    o1, o2, o3 = D_RW, 2 * D_RW, 3 * D_RW
    o4 = o3 + N_DIR * RW_LORA_W
    o5 = o4 + N_DIR * RW_LORA_A
    r, k, v, wd, ad, gd = jnp.split(p, [o1, o2, o3, o4, o5], axis=-1)
    heads = lambda t: t.reshape(B, T, H_RW, HEAD_DIM)
    r, k, v = heads(r), heads(k), heads(v)
    if rope:
        r, k = axial_rope(r), axial_rope(k)
    wd = wd.reshape(B, T, N_DIR, RW_LORA_W)
    ad = ad.reshape(B, T, N_DIR, RW_LORA_A)
    w_pre = (w0 + jnp.einsum('btnr,nrc->btnc', jnp.tanh(wd), w_up)).astype(jnp.float32)
    decay = jnp.exp(-jnp.exp(-jax.nn.softplus(-w_pre) - 0.5)).reshape(B, T, N_DIR, H_RW, HEAD_DIM)
    a = jax.nn.sigmoid((a0 + jnp.einsum('btnr,nrc->btnc', ad, a_up)).astype(jnp.float32))
    a = a.reshape(B, T, N_DIR, H_RW, HEAD_DIM)
    g = jax.nn.sigmoid(gd) @ g_up
    rf, kf, vf = r.astype(jnp.float32), k.astype(jnp.float32), v.astype(jnp.float32)
    kk = kf * k_k.reshape(H_RW, HEAD_DIM)
    kk = kk / jnp.maximum(jnp.sqrt(jnp.sum(kk * kk, axis=-1, keepdims=True)), 1e-12)
    akk = kk[:, :, None] * a
    kw = kf[:, :, None] * (1 + (a - 1) * k_a.reshape(H_RW, HEAD_DIM))
    return rf, kf, vf, kk, decay, akk, kw, g


def rwkv_scan(S0, r, decay, kk, akk, kw, v, reverse):
    tm = lambda t: jnp.swapaxes(t, 0, 1)

    def step(S, inp):
        r_t, w_t, kk_t, akk_t, k_t, v_t = inp
        S = (S * w_t[:, :, None, :]
             - jnp.einsum('bhvk,bhk->bhv', S, kk_t)[..., None] * akk_t[:, :, None, :]
             + v_t[..., None] * k_t[:, :, None, :])
        return S, jnp.einsum('bhvk,bhk->bhv', S, r_t)

    S, o = lax.scan(step, S0, tuple(map(tm, (r, decay, kk, akk, kw, v))), reverse=reverse)
    return S, jnp.swapaxes(o, 0, 1)


def rwkv_finish(o, r, k, v, g, r_k, ln_g, ln_b):
    B, T = o.shape[:2]
    mu = jnp.mean(o, axis=-1, keepdims=True)
    var = jnp.mean(jnp.square(o - mu), axis=-1, keepdims=True)
    y = ((o - mu) * lax.rsqrt(var + RW_LN_EPS)).reshape(B, T, D_RW) * ln_g + ln_b
    bonus = jnp.sum(r * k * r_k, axis=-1, keepdims=True) * v
    return ((y + bonus.reshape(B, T, D_RW)) * g).astype(g.dtype)


def token_mixer(hx, hc, w_in, na_q_g, na_k_g, na_rpb, rw_mu_prev, rw_mu_next, rw_w0, rw_w_up, rw_a0,
                rw_a_up, rw_g_up, rw_k_k, rw_k_a, rw_r_k, rw_ln_g, rw_ln_b, w_out, need_ctx):
    px, pc = hx @ w_in, hc @ w_in
    qx, kx, vx = na_heads(px[..., :3 * D_NA], na_q_g, na_k_g)
    qc, kc, vc = na_heads(pc[..., :3 * D_NA], na_q_g, na_k_g)
    na_x = na_latent(qx, kx, vx, kc, vc, na_rpb)

    rw_args = (rw_mu_prev, rw_mu_next, rw_w0, rw_w_up, rw_a0, rw_a_up, rw_g_up, rw_k_k, rw_k_a)
    rl, kl, vl, kkl, decl, akkl, kwl, gl = rwkv_streams(px[..., 3 * D_NA:], *rw_args, rope=True)
    rc, krc, vrc, kkc, decc, akkc, kwc, gc = rwkv_streams(pc[..., 3 * D_NA:], *rw_args, rope=False)
    B = hx.shape[0]
    o_lat, o_ctx = [], []
    for d in range(N_DIR):
        S0 = jnp.zeros((B, H_RW, HEAD_DIM, HEAD_DIM), jnp.float32)
        S_c, oc = rwkv_scan(S0, rc, decc[:, :, d], kkc, akkc[:, :, d], kwc[:, :, d], vrc, d == 1)
        _, ol = rwkv_scan(S_c, rl, decl[:, :, d], kkl, akkl[:, :, d], kwl[:, :, d], vl, d == 1)
        o_lat.append(ol)
        o_ctx.append(oc)
    rw_x = rwkv_finish(o_lat[0] + o_lat[1], rl, kl, vl, gl, rw_r_k, rw_ln_g, rw_ln_b)
    yx = jnp.concatenate([na_x, rw_x], axis=-1) @ w_out
    if not need_ctx:
        return yx, None
    rw_c = rwkv_finish(o_ctx[0] + o_ctx[1], rc, krc, vrc, gc, rw_r_k, rw_ln_g, rw_ln_b)
    yc = jnp.concatenate([na_context(qc, kc, vc), rw_c], axis=-1) @ w_out
    return yx, yc


def hier_moe(h, wg, bg, we, be, w1, w3, w2):
    B, T, D = h.shape
    hf = h.reshape(B * T, D)
    g_logits = (hf @ wg + bg).astype(jnp.float32)
    g_prob = jax.nn.softmax(g_logits, axis=-1)
    g_sel = jnp.argmax(g_logits, axis=-1)
    p_group = jnp.take_along_axis(g_prob, g_sel[:, None], axis=-1)
    e_all = jnp.einsum('nd,gde->nge', hf, we) + be
    e_logits = jnp.take_along_axis(e_all, g_sel[:, None, None], axis=1)[:, 0].astype(jnp.float32)
    top_v, top_i = lax.top_k(e_logits, MOE_TOPK)
    top_w = jax.nn.softmax(top_v, axis=-1) * p_group
    expert_id = g_sel[:, None] * MOE_PER_GROUP + top_i
    combine = jnp.sum(jax.nn.one_hot(expert_id, N_EXPERTS, dtype=jnp.float32) * top_w[..., None], axis=1)
    combine = combine.astype(h.dtype)
    out = jnp.zeros_like(hf)
    for e in range(N_EXPERTS):
        he = jax.nn.silu(hf @ w1[e]) * (hf @ w3[e])
        out = out + combine[:, e:e + 1] * (he @ w2[e])
    return out.reshape(B, T, D)


def setup_inputs(seed: int = 0) -> dict:
    key = jax.random.key(seed)
    ks = iter(jax.random.split(key, 40))
    f32 = jnp.float32
    nrm = lambda shape, s: jax.random.normal(next(ks), shape, f32) * s
    L, D = DEPTH, D_MODEL
    w0_base = jnp.tile(-6.0 + 5.0 * jnp.linspace(0.0, 1.0, HEAD_DIM, dtype=f32) ** 0.9, H_RW)
    return {
        'x': nrm((BATCH, SEQ, D), 1.0),
        'c': nrm((BATCH, D), 1.0),
        'ctx': nrm((BATCH, CTX_LEN, D), 1.0),
        'c_ctx': nrm((D,), 1.0),
        'w_mod': nrm((L, D, 6 * D), 0.5 * D ** -0.5),
        'b_mod': nrm((L, 6 * D), 0.02),
        'norm1_g': 1.0 + nrm((L, D), 0.02),
        'norm2_g': 1.0 + nrm((L, D), 0.02),
        'w_in': nrm((L, D, D_IN), D ** -0.5),
        'na_q_g': 1.0 + nrm((L, HEAD_DIM), 0.02),
        'na_k_g': 1.0 + nrm((L, HEAD_DIM), 0.02),
        'na_rpb': nrm((L, H_NA, 2 * NA_KH - 1, 2 * NA_KW - 1), 0.2),
        'rw_mu_prev': 0.5 * jax.random.uniform(next(ks), (L, D_RW_IN), f32),
        'rw_mu_next': 0.5 * jax.random.uniform(next(ks), (L, D_RW_IN), f32),
        'rw_w0': w0_base + nrm((L, N_DIR, D_RW), 0.1),
        'rw_w_up': nrm((L, N_DIR, RW_LORA_W, D_RW), 0.1),
        'rw_a0': nrm((L, N_DIR, D_RW), 0.1),
        'rw_a_up': nrm((L, N_DIR, RW_LORA_A, D_RW), RW_LORA_A ** -0.5),
        'rw_g_up': nrm((L, RW_LORA_G, D_RW), RW_LORA_G ** -0.5),
        'rw_k_k': 0.85 + nrm((L, D_RW), 0.02),
        'rw_k_a': 1.0 + nrm((L, D_RW), 0.02),
        'rw_r_k': nrm((L, H_RW, HEAD_DIM), 0.1),
        'rw_ln_g': 1.0 + nrm((L, D_RW), 0.02),
        'rw_ln_b': nrm((L, D_RW), 0.02),
        'w_out': nrm((L, D_MIX, D), D_MIX ** -0.5),
        'moe_wg': nrm((L, D, MOE_GROUPS), D ** -0.5),
        'moe_bg': nrm((L, MOE_GROUPS), 0.01),
        'moe_we': nrm((L, MOE_GROUPS, D, MOE_PER_GROUP), D ** -0.5),
        'moe_be': nrm((L, MOE_GROUPS, MOE_PER_GROUP), 0.01),
        'moe_w1': nrm((L, N_EXPERTS, D, D_EXPERT), D ** -0.5),
        'moe_w3': nrm((L, N_EXPERTS, D, D_EXPERT), D ** -0.5),
        'moe_w2': nrm((L, N_EXPERTS, D_EXPERT, D), D_EXPERT ** -0.5),
    }


def reference(x, c, ctx, c_ctx, w_mod, b_mod, norm1_g, norm2_g, w_in, na_q_g, na_k_g, na_rpb,
              rw_mu_prev, rw_mu_next, rw_w0, rw_w_up, rw_a0, rw_a_up, rw_g_up, rw_k_k, rw_k_a, rw_r_k,
              rw_ln_g, rw_ln_b, w_out, moe_wg, moe_bg, moe_we, moe_be, moe_w1, moe_w3, moe_w2):
    for l in range(DEPTH):
        last = l == DEPTH - 1
        mod_x = jnp.split((jax.nn.silu(c) @ w_mod[l] + b_mod[l])[:, None, :], 6, axis=-1)
        mod_c = jnp.split(jax.nn.silu(c_ctx) @ w_mod[l] + b_mod[l], 6, axis=-1)
        hx = modulate(rmsnorm(x, norm1_g[l]), mod_x[0], mod_x[1])
        hc = modulate(rmsnorm(ctx, norm1_g[l]), mod_c[0], mod_c[1])
        yx, yc = token_mixer(hx, hc, w_in[l], na_q_g[l], na_k_g[l], na_rpb[l], rw_mu_prev[l], rw_mu_next[l],
                             rw_w0[l], rw_w_up[l], rw_a0[l], rw_a_up[l], rw_g_up[l], rw_k_k[l], rw_k_a[l],
                             rw_r_k[l], rw_ln_g[l], rw_ln_b[l], w_out[l], not last)
        x = x + mod_x[2] * yx
        hx2 = modulate(rmsnorm(x, norm2_g[l]), mod_x[3], mod_x[4])
        x = x + mod_x[5] * hier_moe(hx2, moe_wg[l], moe_bg[l], moe_we[l], moe_be[l], moe_w1[l], moe_w3[l], moe_w2[l])
        if not last:
            ctx = ctx + mod_c[2] * yc
            hc2 = modulate(rmsnorm(ctx, norm2_g[l]), mod_c[3], mod_c[4])
            ctx = ctx + mod_c[5] * hier_moe(hc2, moe_wg[l], moe_bg[l], moe_we[l], moe_be[l], moe_w1[l], moe_w3[l], moe_w2[l])
    return x
```

```python
import contextlib
import numpy as np
import concourse.bass as bass
import concourse.mybir as mybir
from concourse.bass_utils import run_bass_kernel_spmd

F32 = mybir.dt.float32
BF16 = mybir.dt.bfloat16
AF = mybir.ActivationFunctionType
ALU = mybir.AluOpType
AX = mybir.AxisListType

D = 1024
SEQ = 2048
CTX = 256
NB = 2
NT = 18
DIN = 3456
DRW = 1920
NEGM = -30000.0
CDEC = 0.6065306597126334
PS_ROWS = 2308
CTX_BASE = 1
LAT_BASE = 259


class Sched:
    def __init__(self, nc, es):
        self.nc = nc
        self.es = es
        self.eng = {"pe": nc.tensor, "act": nc.scalar, "dve": nc.vector, "pool": nc.gpsimd, "sp": nc.sync}
        self.sem = {}
        self.cnt = {}
        for e in ("pe", "act", "dve", "pool"):
            self.sem[e] = es.enter_context(nc.semaphore("s_" + e))
            self.cnt[e] = 0
        self.waited = {}
        self.last_w = {}
        self.reads = {}
        self.nops = 0

    def _src_sem(self, src):
        if src not in self.sem:
            self.sem[src] = self.es.enter_context(self.nc.semaphore("s_dma%d" % len(self.sem)))
            self.cnt[src] = 0
        return self.sem[src]

    def _wait(self, eng, deps, small):
        need = {}
        for (src, c, sm) in deps:
            if src == eng:
                if eng == "pe" or eng == "sp":
                    continue
            if need.get(src, 0) < c:
                need[src] = c
        for src, c in need.items():
            if self.waited.get((eng, src), 0) >= c:
                continue
            self.eng[eng].wait_ge(self._src_sem(src), c)
            self.waited[(eng, src)] = c

    def _deps(self, r, w):
        deps = []
        for k in r:
            if k in self.last_w:
                deps.append(self.last_w[k])
        for k in w:
            if k in self.last_w:
                deps.append(self.last_w[k])
            deps.extend(self.reads.get(k, ()))
        return deps

    def _record(self, me, r, w):
        for k in r:
            self.reads.setdefault(k, []).append(me)
        for k in w:
            self.last_w[k] = me
            self.reads[k] = []

    def op(self, eng, fn, r=(), w=(), small=False, signal=True):
        self._wait(eng, self._deps(r, w), small)
        inst = fn(self.eng[eng])
        if signal:
            inst.then_inc(self.sem[eng], 1)
            self.cnt[eng] += 1
            me = (eng, self.cnt[eng], small)
        else:
            me = (eng, self.cnt[eng] + 1, small)
        self._record(me, r, w)
        self.nops += 1
        return inst

    def dma(self, q, out, in_, r=(), w=(), chan=None):
        src = ("dma", chan if chan is not None else (w[0] if w else r[0]))
        sem = self._src_sem(src)
        deps = self._deps(r, w)
        if self.cnt[src] > 0:
            deps.append((src, self.cnt[src], False))
        self._wait(q, deps, False)
        self.eng[q].dma_start(out=out, in_=in_).then_inc(sem, 16)
        self.cnt[src] += 16
        me = (src, self.cnt[src], False)
        self._record(me, r, w)
        self.nops += 1

    def barrier(self):
        snap = [(src, c) for src, c in self.cnt.items() if c > 0]
        for e in ("pe", "act", "dve", "pool", "sp"):
            for src, c in snap:
                if src == e:
                    continue
                if self.waited.get((e, src), 0) >= c:
                    continue
                self.eng[e].wait_ge(self._src_sem(src), c)
                self.waited[(e, src)] = c

    def wait_all(self, eng="sp"):
        for src, c in self.cnt.items():
            if c > 0 and src != eng:
                self.eng[eng].wait_ge(self._src_sem(src), c)


def build(dbg=None, stages=("P", "NA", "RW", "O", "MOE"), nb=NB):
    nc = bass.Bass("TRN2", target_bir_lowering=False)

    declared = []

    def din(name, shape, dt=F32):
        declared.append(name)
        return nc.dram_tensor(name, list(shape), dt, kind="ExternalInput").ap()

    x_d = din("x", [NB, SEQ, D])
    ctx_d = din("ctx", [NB, CTX, D])
    cT_d = din("cT", [128, 8, 3])
    wmod_d = din("w_mod", [D, 6 * D])
    bmodT_d = din("bmodT", [128, 48])
    g1T_d = din("g1T", [128, 8])
    g2T_d = din("g2T", [128, 8])
    win_d = din("w_in", [D, DIN])
    qg_d = din("na_q_g", [64])
    kg_d = din("na_k_g", [64])
    mtab_d = din("mtab", [128, 32, 480])
    rmask_d = din("rmask", [2, 32 * 128])
    sel2_d = din("sel2", [2, 128])
    rope_d = din("rope", [128, 16, 2, 32])
    tri_d = din("tri", [128, 4, 128])
    ident_d = din("ident", [128, 128])
    blk_d = din("blk", [128, 4, 128])
    mup_d = din("rw_mu_prev", [DRW])
    mun_d = din("rw_mu_next", [DRW])
    w0_d = din("rw_w0", [2 * 512])
    wup_d = din("rw_w_up", [2, 64, 512])
    a0_d = din("rw_a0", [2 * 512])
    aup_d = din("rw_a_up", [2, 64, 512])
    gup_d = din("rw_g_up", [128, 512])
    kk_d = din("rw_k_k", [512])
    ka_d = din("rw_k_a", [512])
    rk_d = din("rw_r_k", [512])
    lng_d = din("rw_ln_g", [512])
    lnb_d = din("rw_ln_b", [512])
    wout_d = din("w_out", [D, D])
    wcat_d = din("wcat", [D, 36])
    bcat_d = din("bcat", [36])
    if "MOE" in stages:
        w1_d = din("moe_w1", [32, D, 512])
        w3_d = din("moe_w3", [32, D, 512])
        w2_d = din("moe_w2", [32, 512, D])
    y_d = nc.dram_tensor("y", [NB, SEQ, D], F32, kind="ExternalOutput").ap()
    pscr_d = nc.dram_tensor("pscr", [NB, PS_ROWS, DRW], F32, kind="Internal").ap()
    dbg_d = {}
    if dbg:
        for name, shape in dbg.items():
            dt_ = F32
            if shape[0] == "bf16":
                dt_, shape = BF16, shape[1:]
            dbg_d[name] = nc.dram_tensor("dbg_" + name, list(shape), dt_, kind="ExternalOutput").ap()

    with contextlib.ExitStack() as es:
        S = Sched(nc, es)

        uid = [0]

        def sb(name, shape, dt=F32, stack=es):
            uid[0] += 1
            return stack.enter_context(nc.sbuf_tensor("s%d_%s" % (uid[0], name), list(shape), dt))

        def ps(name, shape, dt=F32, stack=es):
            uid[0] += 1
            shape = list(shape)
            esz = 4 if dt == F32 else 2
            per = 1
            for d_ in shape[1:]:
                per *= d_
            inner = 1
            for d_ in shape[2:]:
                inner *= d_
            orig1 = shape[1]
            while (per * esz) % 2048 != 0:
                shape[1] += 1
                per = shape[1] * inner
            t_ = stack.enter_context(nc.psum_tensor("p%d_%s" % (uid[0], name), shape, dt))
            if shape[1] == orig1:
                return t_
            return t_[:][:, 0:orig1]

        ident_f = sb("ident_f", [128, 128])
        ident_b = sb("ident_b", [128, 128], BF16)
        tri_f = sb("tri_f", [128, 4, 128])
        ones_b = sb("ones_b", [128, 128], BF16)
        ones_f = sb("ones_f", [128, 128])
        modT = sb("modT", [128, 48, 3])
        gsT = sb("gsT", [128, 2, 8, 3])
        g1T = sb("g1T", [128, 8])
        g2T = sb("g2T", [128, 8])
        epsc = sb("epsc", [128, 1])
        S.dma("sp", ident_f[:], ident_d[:, :], w=["ident_f"])
        S.dma("sp", tri_f[:], tri_d[:, :, :], w=["tri_f"])
        S.dma("sp", g1T[:], g1T_d[:, :], w=["g1T"])
        S.dma("sp", g2T[:], g2T_d[:, :], w=["g2T"])
        S.op("dve", lambda e: e.tensor_copy(out=ident_b[:], in_=ident_f[:]), r=["ident_f"], w=["ident_b"])
        S.op("dve", lambda e: e.memset(ones_b[:], 1.0), w=["ones_b"])
        S.op("dve", lambda e: e.memset(ones_f[:], 1.0), w=["ones_f"])
        S.op("dve", lambda e: e.memset(epsc[:], 1e-6), w=["epsc"], small=True)

        import os
        KPRE = int(os.environ.get('KPRE', 9))
        with contextlib.ExitStack() as st:
            cT = sb("cT", [128, 8, 3], stack=st)
            bmT = sb("bmT", [128, 48], stack=st)
            slab = [sb("slab%d" % i, [128, 8, 1024], stack=st) for i in range(2)]
            ps_mod = ps("ps_mod", [128, 48, 4], stack=st)
            S.dma("sp", cT[:], cT_d[:, :, :], w=["cT"])
            S.dma("sp", bmT[:], bmodT_d[:, :], w=["bmT"])
            S.op("act", lambda e: e.activation(out=cT[:], in_=cT[:], func=AF.Silu), r=["cT"], w=["cT"], small=True)
            wv = wmod_d.rearrange("(kc p) (s f) -> s p kc f", p=128, f=1024)
            for s in range(6 if KPRE >= 1 else 0):
                sl = slab[s % 2]
                S.dma("sp" if s % 2 == 0 else "act", sl[:], wv[s], w=[("slab", s % 2)])
                for f in range(8):
                    for kc in range(8):
                        S.op("pe", lambda e, f=f, kc=kc, sl=sl, s=s: e.matmul(
                            ps_mod[:, s * 8 + f, 0:3], lhsT=sl[:, kc, f * 128:(f + 1) * 128], rhs=cT[:, kc, :],
                            start=(kc == 0), stop=(kc == 7)),
                            r=[("slab", s % 2), "cT"], w=["ps_mod"], signal=(kc == 7))
            for j in range(3):
                S.op("dve", lambda e, j=j: e.tensor_tensor(out=modT[:, :, j], in0=ps_mod[:, :, j], in1=bmT[:],
                                                            op=ALU.add), r=["bmT"], w=["modT", "ps_mod"], small=True)
            for m, gT in ((0, g1T), (1, g2T)):
                sc = modT[:, 8 + 24 * m: 16 + 24 * m, :]
                S.op("dve", lambda e, m=m, sc=sc, gT=gT: e.scalar_tensor_tensor(
                    out=gsT[:, m], in0=sc, scalar=1.0, in1=gT[:, :].unsqueeze(2).to_broadcast([128, 8, 3]),
                    op0=ALU.add, op1=ALU.mult), r=["modT", "g1T", "g2T"], w=["gsT"], small=True)
            S.barrier()
        if dbg and "modT" in dbg:
            S.dma("sp", dbg_d["modT"][:, :, :], modT[:], r=["modT"], chan="dbg")

        for b in range(nb if KPRE >= 3 else 0):
            build_batch(nc, S, es, sb, ps, b, locals(), dbg, dbg_d, stages)

        S.wait_all("sp")
    nc._declared_inputs = declared
    return nc


def build_batch(nc, S, es, sb, ps, b, G, dbg, dbg_d, stages):
    ident_f, ident_b, tri_f, ones_b, ones_f = (G[k] for k in ("ident_f", "ident_b", "tri_f", "ones_b", "ones_f"))
    modT, gsT, epsc = G["modT"], G["gsT"], G["epsc"]
    x_d, ctx_d, y_d, pscr_d = G["x_d"], G["ctx_d"], G["y_d"], G["pscr_d"]

    def tile_src(i):
        return ctx_d[b, i * 128:(i + 1) * 128, :] if i < 2 else x_d[b, (i - 2) * 128:(i - 1) * 128, :]

    def rms_rstd(eng_sq, xt_ap, rstd, junk, key_x, key_r):
        S.op("act", lambda e: e.activation(out=junk, in_=xt_ap, func=AF.Square, accum_out=rstd),
             r=[key_x], w=[key_r, "junk"], small=True)
        S.op("act", lambda e: e.activation(out=rstd, in_=rstd, func=AF.Sqrt, bias=epsc[:, 0:1], scale=1.0 / D),
             r=[key_r, "epsc"], w=[key_r], small=True)
        S.op("dve", lambda e: e.reciprocal(out=rstd, in_=rstd), r=[key_r], w=[key_r], small=True)

    with contextlib.ExitStack() as sbat:
        mixT = sb("mixT", [128, 8, SEQ], BF16, stack=sbat)
        hx2T = mixT
        comb = sb("comb", [128, 16, 32], stack=sbat)
        big32 = sb("big32", [128, 32], stack=sbat)
        G = dict(G)
        G["big32"] = big32
        with contextlib.ExitStack() as sm:
            if "P" in stages:
                stage_P_NA(nc, S, sm, sb, ps, b, G, dbg, dbg_d, stages, tile_src, rms_rstd, mixT)
            if "RW" in stages:
                stage_RW(nc, S, sb, ps, b, G, dbg, dbg_d, mixT)
            if "O" in stages:
                stage_O(nc, S, sb, ps, b, G, dbg, dbg_d, mixT, hx2T, comb, rms_rstd)
            S.barrier()
        if "MOE" in stages:
            stage_MOE(nc, S, sb, ps, b, G, dbg, dbg_d, hx2T, comb)
        S.barrier()


def stage_P_NA(nc, S, sm, sb, ps, b, G, dbg, dbg_d, stages, tile_src, rms_rstd, mixT):
    ident_b, ident_f, ones_b = G["ident_b"], G["ident_f"], G["ones_b"]
    modT, gsT = G["modT"], G["gsT"]
    pscr_d = G["pscr_d"]
    with contextlib.ExitStack() as st:
        qT = sb("qT", [128, 4, SEQ], BF16, stack=st)
        kT = sb("kT", [128, 4, CTX + SEQ], BF16, stack=st)
        vE = sb("vE", [128, NT, 8, 65], BF16, stack=st)
        stage_P(nc, S, sb, ps, b, G, dbg, dbg_d, tile_src, rms_rstd, qT, kT, vE)
        if "NA" in stages:
            stage_NA(nc, S, sb, ps, b, G, dbg, dbg_d, qT, kT, vE, mixT)
        S.barrier()


def stage_P(nc, S, sb, ps, b, G, dbg, dbg_d, tile_src, rms_rstd, qT, kT, vE):
    import os
    ident_b, ident_f, ones_b = G["ident_b"], G["ident_f"], G["ones_b"]
    modT, gsT = G["modT"], G["gsT"]
    pscr_d = G["pscr_d"]
    with contextlib.ExitStack() as st:
        w_in = sb("w_in", [128, 8, DIN], BF16, stack=st)
        qgr = sb("qgr", [128, 64], stack=st)
        kgr = sb("kgr", [128, 64], stack=st)
        xt = [sb("xt%d" % i, [128, D], stack=st) for i in range(2)]
        xn = sb("xn", [128, D], BF16, stack=st)
        junk = sb("junk", [128, D], BF16, stack=st)
        hxT = sb("hxT", [128, 8, 128], BF16, stack=st)
        rstd = sb("rstd", [128, 2], stack=st)
        prw = [sb("prw%d" % i, [128, DRW], stack=st) for i in range(2)]
        qk32 = sb("qk32", [128, 2, 512], stack=st)
        qkn = sb("qkn", [128, 2, 512], BF16, stack=st)
        ssq = sb("ssq", [128, 16], stack=st)
        zrow = sb("zrow", [4, DRW], stack=st)
        eps64 = sb("eps64", [128, 1], stack=st)
        ps_t = ps("ps_t", [128, 8, 128], BF16, stack=st)
        ps_p = [ps("ps_p%d" % i, [128, 512], stack=st) for i in range(4)]
        ps_qt = ps("ps_qt", [128, 8, 128], BF16, stack=st)

        wv = G["win_d"].rearrange("(kc p) n -> p kc n", p=128)
        for kc in range(8):
            S.dma("pool", w_in[:, kc, :], wv[:, kc, :], w=[("w_in", kc)], chan=("w_in", kc % 4))
        S.dma("sp", qgr[:], G["qg_d"].partition_broadcast(128), w=["qgr"])
        S.dma("sp", kgr[:], G["kg_d"].partition_broadcast(128), w=["kgr"])
        S.op("dve", lambda e: e.tensor_scalar(out=qgr[:], in0=qgr[:], scalar1=0.125, scalar2=None, op0=ALU.mult),
             r=["qgr"], w=["qgr"], small=True)
        S.op("dve", lambda e: e.memset(eps64[:], 1e-6), w=["eps64"], small=True)
        S.op("pool", lambda e: e.memset(vE[:, :, :, 64:65], 1.0), w=["vE"])
        S.op("pool", lambda e: e.memset(zrow[:], 0.0), w=["zrow"])
        for r0 in (0, 257, 258, 2307):
            S.dma("sp", pscr_d[b, r0:r0 + 1, :], zrow[0:1, :], r=["zrow"], chan="zr")

        import os
        for i in range(int(os.environ.get('KNT', NT))):
            xb_ = xt[i % 2]
            kx = ("xt", i % 2)
            S.dma("sp", xb_[:], tile_src(i), w=[kx])
            rs = rstd[:, 0:1]
            rms_rstd("act", xb_[:], rs, junk[:], kx, "rstd")
            KSUB = int(os.environ.get('KSUB', 9))
            if KSUB < 2:
                continue
            S.op("dve", lambda e: e.tensor_scalar(out=xn[:], in0=xb_[:], scalar1=rs, scalar2=None, op0=ALU.mult),
                 r=[kx, "rstd"], w=["xn"])
            for j in range(8):
                S.op("pe", lambda e, j=j: e.transpose(out=ps_t[:, j, :], in_=xn[:, j * 128:(j + 1) * 128],
                                                      identity=ident_b[:]), r=["xn", "ident_b"], w=["ps_t"],
                     signal=(j == 7))
            mb = 2 if i < 2 else b
            KEV = os.environ.get('KEV', 'AD')
            for j in range(8):
                eng = "act" if i % 2 == 0 else "dve"
                if eng == "act":
                    S.op("act", lambda e, j=j: e.activation(out=hxT[:, j, :], in_=ps_t[:, j, :], func=AF.Identity,
                                                            bias=modT[:, j, mb:mb + 1], scale=gsT[:, 0, j, mb:mb + 1]),
                         r=["modT", "gsT"], w=[("hxT", j), "ps_t"])
                else:
                    S.op("dve", lambda e, j=j: e.tensor_scalar(out=hxT[:, j, :], in0=ps_t[:, j, :],
                                                               scalar1=gsT[:, 0, j, mb:mb + 1],
                                                               scalar2=modT[:, j, mb:mb + 1], op0=ALU.mult, op1=ALU.add),
                         r=["modT", "gsT"], w=[("hxT", j), "ps_t"])
            if KSUB < 3:
                continue
            pr = prw[i % 2]
            kpr = ("prw", i % 2)
            for n in range(7):
                ncol = 512 if n < 6 else DIN - 6 * 512
                pp = ps_p[n % 4]
                kp = ("ps_p", n % 4)
                for kc in range(8):
                    S.op("pe", lambda e, n=n, kc=kc, pp=pp, ncol=ncol: e.matmul(
                        pp[:, 0:ncol], lhsT=hxT[:, kc, :], rhs=w_in[:, kc, n * 512:n * 512 + ncol],
                        start=(kc == 0), stop=(kc == 7)), r=[("hxT", kc), ("w_in", kc)], w=[kp], signal=(kc == 7))
                if n < 2:
                    if (n == 0 and i < 2) or KSUB < 4:
                        continue
                    gr = qgr if n == 0 else kgr
                    S.op("act", lambda e, n=n, pp=pp: e.activation(out=qk32[:, n, :], in_=pp[:], func=AF.Copy),
                         w=[("qk32", n), kp])
                    S.op("dve", lambda e, n=n, pp=pp: e.tensor_tensor(out=junk[:, 0:512], in0=pp[:], in1=qk32[:, n, :],
                                                                      op=ALU.mult), r=[("qk32", n)], w=["junk", kp])
                    sq = ssq[:, n * 8:(n + 1) * 8]
                    S.op("dve", lambda e, sq=sq: e.tensor_reduce(out=sq, in_=junk[:, 0:512].rearrange("p (h d) -> p h d", d=64),
                                                                 axis=AX.X, op=ALU.add), r=["junk"], w=["ssq"], small=True)
                    S.op("act", lambda e, sq=sq: e.activation(out=sq, in_=sq, func=AF.Sqrt, bias=eps64[:, 0:1], scale=1.0 / 64),
                         r=["ssq", "eps64"], w=["ssq"], small=True)
                    S.op("dve", lambda e, sq=sq: e.reciprocal(out=sq, in_=sq), r=["ssq"], w=["ssq"], small=True)
                    q3 = qk32[:, n, :].rearrange("p (h d) -> p h d", d=64)
                    S.op("dve", lambda e, sq=sq, q3=q3: e.tensor_tensor(out=q3, in0=q3, in1=sq.unsqueeze(2).to_broadcast([128, 8, 64]),
                                                                        op=ALU.mult), r=["ssq", ("qk32", n)], w=[("qk32", n)])
                    S.op("pool", lambda e, n=n, q3=q3, gr=gr: e.tensor_tensor(
                        out=qkn[:, n, :].rearrange("p (h d) -> p h d", d=64), in0=q3,
                        in1=gr[:, :].unsqueeze(1).to_broadcast([128, 8, 64]), op=ALU.mult),
                        r=[("qk32", n), "qgr", "kgr"], w=[("qkn", n)])
                    for pr_ in range(4):
                        S.op("pe", lambda e, n=n, pr_=pr_: e.transpose(out=ps_qt[:, n * 4 + pr_, :],
                                                                       in_=qkn[:, n, pr_ * 128:(pr_ + 1) * 128],
                                                                       identity=ident_b[:]),
                             r=[("qkn", n), "ident_b"], w=["ps_qt"], signal=(pr_ == 3))
                    if n == 0:
                        S.op("act", lambda e, i=i: e.activation(out=qT[:, :, (i - 2) * 128:(i - 1) * 128], in_=ps_qt[:, 0:4, :],
                                                                func=AF.Copy), w=["qT", "ps_qt"])
                    else:
                        S.op("act", lambda e, i=i: e.activation(out=kT[:, :, i * 128:(i + 1) * 128], in_=ps_qt[:, 4:8, :],
                                                                func=AF.Copy), w=["kT", "ps_qt"])
                elif n == 2:
                    S.op("act", lambda e, i=i, pp=pp: e.activation(out=vE[:, i, :, 0:64],
                                                                   in_=pp[:].rearrange("p (h d) -> p h d", d=64),
                                                                   func=AF.Copy), w=["vE", kp])
                else:
                    c0 = (n - 3) * 512
                    eng = "dve" if n % 2 == 1 else "act"
                    if eng == "dve":
                        S.op("dve", lambda e, pp=pp, c0=c0, ncol=ncol, pr=pr: e.tensor_copy(out=pr[:, c0:c0 + ncol], in_=pp[:, 0:ncol]),
                             w=[kpr, kp])
                    else:
                        S.op("act", lambda e, pp=pp, c0=c0, ncol=ncol, pr=pr: e.activation(out=pr[:, c0:c0 + ncol], in_=pp[:, 0:ncol],
                                                                                           func=AF.Copy), w=[kpr, kp])
            t0 = CTX_BASE + 128 * i if i < 2 else LAT_BASE + 128 * (i - 2)
            S.dma("sp", pscr_d[b, t0:t0 + 128, :], pr[:], r=[kpr], w=[("pscr", i)], chan=("pscw", i % 2))
        if dbg and "kT" in dbg and b == 0:
            S.dma("sp", dbg_d["kT"][:, :, :], kT[:], r=["kT"], chan="dbg")
            S.dma("sp", dbg_d["qT"][:, :, :], qT[:], r=["qT"], chan="dbg")
            S.dma("sp", dbg_d["vE"][:, :, :, :], vE[:], r=["vE"], chan="dbg")
        S.barrier()


def stage_NA(nc, S, sb, ps, b, G, dbg, dbg_d, qT, kT, vE, mixT):
    ident_b, ones_b = G["ident_b"], G["ones_b"]
    with contextlib.ExitStack() as st:
        mtab = sb("mtab", [128, 32, 480], BF16, stack=st)
        rmask = sb("rmask", [66, 32 * 128], BF16, stack=st)
        sel2 = sb("sel2", [66, 128], BF16, stack=st)
        natok = sb("natok", [128, 512], BF16, stack=st)
        pT = [sb("pT%d" % i, [128, 10, 128], BF16, stack=st) for i in range(2)]
        rden = sb("rden", [128, 2], stack=st)
        psw = [ps("psw%d" % i, [128, 1536], stack=st) for i in range(2)]
        psO = ps("psO", [128, 128], stack=st)
        psT = ps("psT", [128, 4, 128], BF16, stack=st)
        S.dma("pool", mtab[:], G["mtab_d"][:, :, :], w=["mtab"])
        for pb_ in (0, 64):
            S.dma("pool", rmask[pb_:pb_ + 2, :], G["rmask_d"][:, :], w=[("rmask", pb_)], chan=("rmaskA", pb_))
            S.dma("pool", sel2[pb_:pb_ + 2, :], G["sel2_d"][:, :], w=[("sel2", pb_)], chan=("rmaskB", pb_))
        r0s = [0, 4, 12, 16]
        import os
        KNA = int(os.environ.get("KNA", 9))
        KNAB = int(os.environ.get("KNAB", 16))
        def emit_scores(rb, cb, h):
            pair, hb = h // 2, (h % 2) * 64
            pw = psw[h % 2]
            kw_ = ("psw", h % 2)
            pt = pT[h % 2]
            kpt = ("pT", h % 2)
            qblk = qT[hb:hb + 64, pair, :].rearrange("p (r c) -> p r c", c=64)[:, 8 * rb:8 * rb + 8, 16 * cb:16 * cb + 16]
            for c in range(2):
                S.op("pe", lambda e, c=c: e.matmul(pw[:, c * 128:(c + 1) * 128], lhsT=kT[hb:hb + 64, pair, c * 128:(c + 1) * 128],
                                                   rhs=qblk, start=True, stop=True), r=["kT", "qT"], w=[kw_], signal=False)
            for sl_ in range(8):
                kr_e = r0s[rb] + 2 * sl_
                ktok = CTX + kr_e * 64
                e0 = 14 - (kr_e - 8 * rb)
                out = pw[:, (2 + sl_) * 128:(3 + sl_) * 128]
                S.op("pe", lambda e, out=out, ktok=ktok: e.matmul(out, lhsT=kT[hb:hb + 64, pair, ktok:ktok + 128], rhs=qblk, start=True, stop=False),
                     r=["kT", "qT"], w=[kw_], signal=False)
                S.op("pe", lambda e, out=out, e0=e0: e.matmul(out, lhsT=ident_b[:, :], rhs=mtab[:, h * 4 + cb, e0 * 16:e0 * 16 + 128], start=False, stop=False),
                     r=["mtab", "ident_b"], w=[kw_], signal=False)
                S.op("pe", lambda e, out=out, sl_=sl_: e.matmul(out, lhsT=sel2[hb:hb + 2, :], rhs=rmask[hb:hb + 2, (rb * 8 + sl_) * 128:(rb * 8 + sl_ + 1) * 128],
                                                                start=False, stop=True), r=[("rmask", hb), ("sel2", hb)], w=[kw_], signal=(sl_ == 7))
            for bk in range(3):
                c0_, c1_ = bk * 512, min(bk * 512 + 512, 1280)
                S.op("act", lambda e, c0_=c0_, c1_=c1_: e.activation(out=pt[:].rearrange("p a b -> p (a b)")[:, c0_:c1_], in_=pw[:, c0_:c1_], func=AF.Exp),
                     w=[kpt, kw_])

        def emit_pv(rb, cb, h):
            pt = pT[h % 2]
            kpt = ("pT", h % 2)
            for sl_ in range(8):
                tile_ = 2 + (r0s[rb] + 2 * sl_) // 2
                S.op("pe", lambda e, sl_=sl_, tile_=tile_: e.matmul(psO[:, 0:65], lhsT=pt[:, 2 + sl_, :], rhs=vE[:, tile_, h, :],
                                                                    start=(sl_ == 0), stop=False), r=[kpt, "vE"], w=["psO"], signal=False)
            for c in range(2):
                S.op("pe", lambda e, c=c: e.matmul(psO[:, 0:65], lhsT=pt[:, c, :], rhs=vE[:, c, h, :],
                                                   start=False, stop=(c == 1)), r=[kpt, "vE"], w=["psO"], signal=(c == 1))
            S.op("dve", lambda e: e.reciprocal(out=rden[:, 0:1], in_=psO[:, 64:65]), w=["rden", "psO"], small=True)
            S.op("dve", lambda e: e.tensor_scalar(out=natok[:, h * 64:(h + 1) * 64], in0=psO[:, 0:64], scalar1=rden[:, 0:1],
                                                  scalar2=None, op0=ALU.mult), r=["rden"], w=["natok", "psO"], small=True)
            if h == 7:
                for pr_ in range(4):
                    S.op("pe", lambda e, pr_=pr_: e.transpose(out=psT[:, pr_, :], in_=natok[:, pr_ * 128:(pr_ + 1) * 128],
                                                              identity=ident_b[:]), r=["natok", "ident_b"], w=["psT"], signal=(pr_ == 3))
                mo = mixT[:, 0:4, :].rearrange("p a (r c) -> p a r c", c=64)[:, :, 8 * rb:8 * rb + 8, 16 * cb:16 * cb + 16]
                S.op("act", lambda e, mo=mo: e.activation(out=mo, in_=psT[:].rearrange("p a (r c) -> p a r c", c=16), func=AF.Copy),
                     w=["mixT_na", "psT"])

        items = [(rb, cb, h) for rb in range(4) for cb in range(4) for h in range(8) if rb * 4 + cb < KNAB]
        emit_scores(*items[0])
        for k_ in range(len(items)):
            if k_ + 1 < len(items):
                emit_scores(*items[k_ + 1])
            emit_pv(*items[k_])
        if dbg and "naT" in dbg and b == 0 and KNA >= 7 and KNAB >= 16:
            S.dma("sp", dbg_d["naT"][:, :, :], mixT[:, 0:4, :], r=["mixT_na"], chan="dbg")
        S.barrier()


def _host_prep(inp):
    f32 = np.float32
    L = 0
    c = inp["c"].astype(f32)
    c_ctx = inp["c_ctx"].astype(f32)
    shared = {}
    shared["w_mod"] = np.ascontiguousarray(inp["w_mod"][L])
    shared["bmodT"] = np.ascontiguousarray(inp["b_mod"][L].reshape(48, 128).T)
    shared["g1T"] = np.ascontiguousarray(inp["norm1_g"][L].reshape(8, 128).T)
    shared["g2T"] = np.ascontiguousarray(inp["norm2_g"][L].reshape(8, 128).T)
    shared["w_in"] = np.ascontiguousarray(inp["w_in"][L])
    shared["na_q_g"] = np.ascontiguousarray(inp["na_q_g"][L])
    shared["na_k_g"] = np.ascontiguousarray(inp["na_k_g"][L])
    rpb = inp["na_rpb"][L].astype(f32)
    j = np.arange(64)[:, None, None, None]
    cb = np.arange(4)[None, :, None, None]
    e = np.arange(30)[None, None, :, None]
    qc = np.arange(16)[None, None, None, :]
    ri = 21 - e + 0 * j + 0 * cb + 0 * qc
    qcol = 16 * cb + qc
    ci = j - qcol + 15 + 0 * e
    cs = np.clip(qcol - 8, 0, 48)
    colok = (j >= cs) & (j < cs + 16)
    ok = (ri >= 0) & (ri < 15) & (ci >= 0) & (ci < 31) & (colok | (e < 0))
    ric = np.clip(ri, 0, 14)
    cic = np.clip(ci, 0, 30)
    mt = np.empty((64, 8, 4, 30, 16), f32)
    for h in range(8):
        mt[:, h] = np.where(ok, rpb[h][ric, cic], f32(NEGM))
    mtsh = np.full_like(mt, f32(NEGM))
    mtsh[:, :, :, 1:, :] = mt[:, :, :, :-1, :]
    shared["mtab"] = np.ascontiguousarray(np.concatenate([mt.reshape(64, 32, 480), mtsh.reshape(64, 32, 480)], axis=0))
    rm = np.full((4, 15, 8), NEGM, f32)
    r0s = [0, 4, 12, 17]
    for rb in range(4):
        for jr in range(15):
            kr = r0s[rb] + jr
            for qr in range(8):
                qrow = rb * 8 + qr
                rs_ = min(max(qrow - 4, 0), 24)
                if rs_ <= kr < rs_ + 8:
                    rm[rb, jr, qr] = 0.0
    rm2 = np.full((2, 4, 8, 8), NEGM, f32)
    r0e = [0, 4, 12, 16]
    for rb in range(4):
        for sl_ in range(8):
            for par in range(2):
                kr = r0e[rb] + 2 * sl_ + par
                for qr in range(8):
                    qrow = rb * 8 + qr
                    rs_ = min(max(qrow - 4, 0), 24)
                    if rs_ <= kr < rs_ + 8:
                        rm2[par, rb, sl_, qr] = 0.0
    shared["rmask"] = np.ascontiguousarray(np.repeat(rm2[:, :, :, :, None], 16, axis=4).reshape(2, 32 * 128))
    sel2 = np.zeros((2, 128), f32)
    sel2[0, 0:64] = 1.0
    sel2[1, 64:128] = 1.0
    shared["sel2"] = sel2
    nf = 16
    pos = np.arange(SEQ)
    inv = (10000.0 ** (-np.arange(nf, dtype=np.float32) / nf)).astype(f32)
    ang = np.stack([(pos // 64).astype(f32)[:, None] * inv, (pos % 64).astype(f32)[:, None] * inv], axis=1)
    cs_ = np.concatenate([np.cos(ang).astype(f32), np.sin(ang).astype(f32)], axis=-1)
    shared["rope"] = np.ascontiguousarray(cs_.reshape(16, 128, 2, 32).transpose(1, 0, 2, 3))
    ii = np.arange(128)
    U = (ii[:, None] < ii[None, :]).astype(f32)
    Ui = (ii[:, None] <= ii[None, :]).astype(f32)
    shared["tri"] = np.ascontiguousarray(np.stack([U, Ui, U.T, Ui.T], axis=1))
    shared["ident"] = np.eye(128, dtype=f32)
    blk = [(ii[:, None] // 16 == ii[None, :] // 16)]
    for l_ in range(3):
        blk.append((ii[:, None] // (32 << l_) == ii[None, :] // (32 << l_)) & (ii[:, None] // (16 << l_) != ii[None, :] // (16 << l_)))
    shared["blk"] = np.ascontiguousarray(np.stack(blk, axis=1).astype(f32))
    for k in ("rw_mu_prev", "rw_mu_next", "rw_w_up", "rw_a_up", "rw_g_up", "rw_k_k", "rw_k_a", "rw_ln_g", "rw_ln_b", "w_out"):
        shared[k] = np.ascontiguousarray(inp[k][L])
    shared["rw_w0"] = np.ascontiguousarray(inp["rw_w0"][L].reshape(-1))
    shared["rw_a0"] = np.ascontiguousarray(inp["rw_a0"][L].reshape(-1))
    shared["rw_r_k"] = np.ascontiguousarray(inp["rw_r_k"][L].reshape(-1))
    we = inp["moe_we"][L]
    shared["wcat"] = np.ascontiguousarray(np.concatenate([inp["moe_wg"][L], we.transpose(1, 0, 2).reshape(D, 32)], axis=1))
    shared["bcat"] = np.ascontiguousarray(np.concatenate([inp["moe_bg"][L], inp["moe_be"][L].reshape(-1)]))
    shared["moe_w1"] = np.ascontiguousarray(inp["moe_w1"][L])
    shared["moe_w3"] = np.ascontiguousarray(inp["moe_w3"][L])
    shared["moe_w2"] = np.ascontiguousarray(inp["moe_w2"][L])
    in_maps = []
    for core in range(8):
        m = dict(shared)
        bs = slice(core * NB, (core + 1) * NB)
        m["x"] = np.ascontiguousarray(inp["x"][bs])
        m["ctx"] = np.ascontiguousarray(inp["ctx"][bs])
        cc = np.stack([c[core * NB], c[core * NB + 1], c_ctx], axis=-1)
        m["cT"] = np.ascontiguousarray(cc.reshape(8, 128, 3).transpose(1, 0, 2))
        in_maps.append(m)
    return in_maps


def kernel(**inputs):
    inp = {k: np.asarray(v) for k, v in inputs.items()}
    in_maps = _host_prep(inp)
    nc = build()
    res = run_bass_kernel_spmd(nc, in_maps, core_ids=list(range(8)))
    out = np.concatenate([r["y"] for r in res.results], axis=0)
    return out.astype(np.float32)


def stage_RW(nc, S, sb, ps, b, G, dbg, dbg_d, mixT):
    import os
    ident_b, ident_f, ones_b, ones_f, tri_f = G["ident_b"], G["ident_f"], G["ones_b"], G["ones_f"], G["tri_f"]
    pscr_d = G["pscr_d"]
    C = CDEC
    with contextlib.ExitStack() as st:
        def T(name, shape, dt=F32):
            return sb(name, shape, dt, stack=st)
        mup, mun = T("mup", [128, DRW]), T("mun", [128, DRW])
        w0r, a0r = T("w0r", [128, 1, 512]), T("a0r", [128, 1, 512])
        kkr, kar, omkar, rkr, lngr, lnbr = (T(n, [128, 512]) for n in ("kkr", "kar", "omkar", "rkr", "lngr", "lnbr"))
        wup, aup, gup = T("wup", [64, 2, 512], BF16), T("aup", [64, 2, 512], BF16), T("gup", [128, 512], BF16)
        rope = T("rope", [128, 16, 2, 32])
        ntri = T("ntri", [128, 4, 128])
        idp = T("idp", [128, 64])
        blkb = T("blkb", [128, 4, 128], BF16)
        oacc = T("oacc", [128, 16, 512])
        bon = T("bon", [128, 16, 512], BF16)
        sgd = T("sgd", [128, 16, 128], BF16)
        Hs = [T("H%d" % d, [128, 4, 64], BF16) for d in range(2)]
        p0, pm, pp = T("p0", [128, DRW]), T("pm", [128, DRW]), T("pp", [128, DRW])
        rk2 = T("rk2", [128, 1024])
        tA, tB = T("tA", [128, 512]), T("tB", [128, 512])
        vb = T("vb", [128, 512], BF16)
        lin = T("lin", [128, 256], BF16)
        lT = T("lT", [128, 3, 128], BF16)
        aa = T("aa", [128, 512])
        lw = pp[:, 1024:1536]
        kkn, akk, kw = T("kkn", [128, 512]), T("akk", [128, 512]), T("kw", [128, 512])
        st8 = T("st8", [128, 32])
        Eneg, Epos, Eex = pm[:, 0:512], pm[:, 512:1024], pm[:, 1024:1536]
        Ehat, cum = pp[:, 0:512], pp[:, 512:1024]
        PCf = T("PCf", [128, 4])
        tok6 = T("tok6", [128, 6, 512], BF16)
        fT = T("fT", [128, 4, 4, 128], BF16)
        prod = T("prod", [128, 5, 8, 128], BF16)
        XY2 = T("XY2", [128, 2, 8, 128], BF16)
        Tt = T("Tt", [128, 8, 128], BF16)
        Zs, nW, Abs_ = T("Zs", [128, 512], BF16), T("nW", [128, 512], BF16), T("Abs", [128, 512], BF16)
        RbT = T("RbT", [128, 4, 128], BF16)
        Gt = T("Gt", [128, 4, 64], BF16)
        rwtok = T("rwtok", [128, 512], BF16)
        pb = [ps("pb%d" % i, [128, 512], stack=st) for i in range(6)]
        pt = [ps("pt%d" % i, [128, 8, 128], BF16, stack=st) for i in range(2)]
        KB = [("pb", i) for i in range(6)]
        KT = [("pt", i) for i in range(2)]

        def op(eng, fn, r=(), w=(), small=False, signal=True):
            return S.op(eng, fn, r=r, w=w, small=small, signal=signal)

        for dst, src in ((mup, G["mup_d"]), (mun, G["mun_d"]), (kkr, G["kk_d"]), (kar, G["ka_d"]), (rkr, G["rk_d"]),
                         (lngr, G["lng_d"]), (lnbr, G["lnb_d"])):
            S.dma("sp", dst[:], src.partition_broadcast(128), w=["rwc"], chan="rwc")
        S.dma("sp", rope[:], G["rope_d"][:, :, :, :], w=["rwc"], chan="rwc")
        S.dma("pool", wup[:], G["wup_d"].rearrange("d k n -> k d n"), w=["rwc2"], chan="rwc2")
        S.dma("pool", aup[:], G["aup_d"].rearrange("d k n -> k d n"), w=["rwc2"], chan="rwc2")
        S.dma("pool", gup[:], G["gup_d"][:, :], w=["rwc2"], chan="rwc2")
        S.dma("pool", blkb[:], G["blk_d"][:, :, :], w=["blkb"], chan="rwc2")
        op("dve", lambda e: e.tensor_scalar(out=omkar[:], in0=kar[:], scalar1=-1.0, scalar2=1.0, op0=ALU.mult, op1=ALU.add),
           r=["rwc"], w=["omkar"])
        op("dve", lambda e: e.tensor_scalar(out=ntri[:], in0=tri_f[:], scalar1=-1.0, scalar2=None, op0=ALU.mult), r=["tri_f"], w=["ntri"])
        op("dve", lambda e: e.tensor_copy(out=idp[0:64, :], in_=ident_f[0:64, 0:64]), r=["ident_f"], w=["idp"])
        op("dve", lambda e: e.tensor_copy(out=idp[64:128, :], in_=ident_f[64:128, 64:128]), r=["ident_f"], w=["idp"])
        op("pool", lambda e: e.memset(oacc[:], 0.0), w=["oacc"])

        nvis = int(os.environ.get("KRW", 99))
        for d in range(int(os.environ.get("KDIR", 2))):
            H = Hs[d]
            kH = ("H", d)
            op("pool", lambda e, H=H: e.memset(H[:], 0.0), w=[kH])
            S.dma("sp", w0r[:, 0, :], G["w0_d"][d * 512:(d + 1) * 512].partition_broadcast(128), w=["w0a0"], chan="rwc")
            S.dma("sp", a0r[:, 0, :], G["a0_d"][d * 512:(d + 1) * 512].partition_broadcast(128), w=["w0a0"], chan="rwc")
            order = list(range(NT)) if d == 0 else [1, 0] + list(range(NT - 1, 1, -1))
            mX, mY, mI = (0, 2, 1) if d == 0 else (2, 0, 3)
            for vi, i in enumerate(order[:nvis]):
                lat = i >= 2
                j = i - 2
                t0 = CTX_BASE + 128 * i if i < 2 else LAT_BASE + 128 * j
                S.dma("sp", p0[:], pscr_d[b, t0:t0 + 128, :], r=[("pscr", i)], w=["p0"])
                S.dma("sp", pm[:], pscr_d[b, t0 - 1:t0 + 127, :], r=[("pscr", i), ("pscr", max(i - 1, 0))], w=["pm", "Eneg", "Epos", "Eex"])
                S.dma("sp", pp[:], pscr_d[b, t0 + 1:t0 + 129, :], r=[("pscr", i), ("pscr", min(i + 1, NT - 1))], w=["pp", "Ehat", "cum", "lw"])
                op("dve", lambda e: e.tensor_tensor(out=pm[:], in0=pm[:], in1=p0[:], op=ALU.subtract), r=["p0"], w=["pm", "Eneg", "Epos", "Eex"])
                op("pool", lambda e: e.tensor_tensor(out=pp[:], in0=pp[:], in1=p0[:], op=ALU.subtract), r=["p0"], w=["pp", "Ehat", "cum", "lw"])
                op("dve", lambda e: e.tensor_tensor(out=pm[:], in0=pm[:], in1=mup[:], op=ALU.mult), r=["rwc"], w=["pm", "Eneg", "Epos", "Eex"])
                op("pool", lambda e: e.tensor_tensor(out=pp[:], in0=pp[:], in1=mun[:], op=ALU.mult), r=["rwc"], w=["pp", "Ehat", "cum", "lw"])
                op("dve", lambda e: e.tensor_tensor(out=p0[:], in0=p0[:], in1=pm[:], op=ALU.add), r=["pm"], w=["p0"])
                op("dve", lambda e: e.tensor_tensor(out=p0[:], in0=p0[:], in1=pp[:], op=ALU.add), r=["pp"], w=["p0"])
                if lat:
                    s5 = p0[:, 0:1024].rearrange("p (h a t f) -> p h a t f", a=2, t=2, f=16)
                    d5 = rk2[:].rearrange("p (h a t f) -> p h a t f", a=2, t=2, f=16)
                    cs = rope[:, j, :, 0:16].unsqueeze(1).to_broadcast([128, 16, 2, 16])
                    sn = rope[:, j, :, 16:32].unsqueeze(1).to_broadcast([128, 16, 2, 16])
                    a4 = tA[:, 0:512].rearrange("p (h a f) -> p h a f", a=2, f=16)
                    b4 = tB[:, 0:512].rearrange("p (h a f) -> p h a f", a=2, f=16)
                    t1, t2 = s5[:, :, :, 0, :], s5[:, :, :, 1, :]
                    op("dve", lambda e: e.tensor_tensor(out=d5[:, :, :, 0, :], in0=t1, in1=cs, op=ALU.mult), r=["p0", "rwc"], w=["rk2a"])
                    op("pool", lambda e: e.tensor_tensor(out=a4, in0=t2, in1=sn, op=ALU.mult), r=["p0", "rwc"], w=["tA"])
                    op("dve", lambda e: e.tensor_tensor(out=d5[:, :, :, 0, :], in0=d5[:, :, :, 0, :], in1=a4, op=ALU.subtract), r=["tA"], w=["rk2a"])
                    op("pool", lambda e: e.tensor_tensor(out=d5[:, :, :, 1, :], in0=t2, in1=cs, op=ALU.mult), r=["p0", "rwc"], w=["rk2b"])
                    op("dve", lambda e: e.tensor_tensor(out=b4, in0=t1, in1=sn, op=ALU.mult), r=["p0", "rwc"], w=["tB"])
                    op("pool", lambda e: e.tensor_tensor(out=d5[:, :, :, 1, :], in0=d5[:, :, :, 1, :], in1=b4, op=ALU.add), r=["tB"], w=["rk2b"])
                    rr, kr_ = rk2[:, 0:512], rk2[:, 512:1024]
                    krk = ["rk2a", "rk2b"]
                else:
                    rr, kr_ = p0[:, 0:512], p0[:, 512:1024]
                    krk = ["p0"]
                v_ = p0[:, 1024:1536]
                KRWL = int(os.environ.get("KRWL", 9))
                if KRWL < 2:
                    continue
                op("act", lambda e: e.activation(out=vb[:], in_=v_, func=AF.Copy), r=["p0"], w=["vb"])
                op("act", lambda e: e.activation(out=lin[:, 0:64], in_=p0[:, 1536 + 64 * d:1600 + 64 * d], func=AF.Tanh), r=["p0"], w=["lin"])
                op("act", lambda e: e.activation(out=lin[:, 64:128], in_=p0[:, 1664 + 64 * d:1728 + 64 * d], func=AF.Copy), r=["p0"], w=["lin"])
                dog = lat and d == 0
                if dog:
                    op("act", lambda e: e.activation(out=sgd[:, j, :], in_=p0[:, 1792:1920], func=AF.Sigmoid), r=["p0"], w=["sgd"])
                op("pe", lambda e: e.transpose(out=pt[0][0:64, 0, :], in_=lin[:, 0:64], identity=ident_b[:]), r=["lin"], w=[KT[0]], signal=False)
                op("pe", lambda e: e.transpose(out=pt[0][0:64, 1, :], in_=lin[:, 64:128], identity=ident_b[:]), r=["lin"], w=[KT[0]])
                op("act", lambda e: e.activation(out=lT[0:64, 0:2, :], in_=pt[0][0:64, 0:2, :], func=AF.Copy), w=["lT", KT[0]])
                op("pe", lambda e: e.matmul(pb[0][:], lhsT=lT[0:64, 0, :], rhs=wup[0:64, d, :], start=True, stop=True), r=["lT", "rwc2"], w=[KB[0]])
                op("pe", lambda e: e.matmul(pb[1][:], lhsT=lT[0:64, 1, :], rhs=aup[0:64, d, :], start=True, stop=True), r=["lT", "rwc2"], w=[KB[1]])
                op("dve", lambda e: e.tensor_tensor(out=lw[:], in0=pb[0][:], in1=w0r[:, 0, :], op=ALU.add), r=["w0a0"], w=["lw", KB[0]])
                op("act", lambda e: e.activation(out=lw[:], in_=lw[:], func=AF.Sigmoid), w=["lw"])
                op("dve", lambda e: e.tensor_tensor(out=aa[:], in0=pb[1][:], in1=a0r[:, 0, :], op=ALU.add), r=["w0a0"], w=["aa", KB[1]])
                op("act", lambda e: e.activation(out=aa[:], in_=aa[:], func=AF.Sigmoid), w=["aa"])
                if KRWL < 3:
                    continue
                op("dve", lambda e: e.tensor_tensor(out=kkn[:], in0=kr_, in1=kkr[:], op=ALU.mult), r=krk + ["rwc"], w=["kkn"])
                op("pool", lambda e: e.tensor_tensor(out=tA[:], in0=kkn[:], in1=kkn[:], op=ALU.mult), r=["kkn"], w=["tA"])
                op("dve", lambda e: e.tensor_reduce(out=st8[:, 0:8], in_=tA[:].rearrange("p (h f) -> p h f", f=64), axis=AX.X, op=ALU.add),
                   r=["tA"], w=["st8"], small=True)
                op("act", lambda e: e.activation(out=st8[:, 0:8], in_=st8[:, 0:8], func=AF.Sqrt), w=["st8"], small=True)
                op("dve", lambda e: e.tensor_scalar(out=st8[:, 0:8], in0=st8[:, 0:8], scalar1=1e-12, scalar2=None, op0=ALU.max), w=["st8"], small=True)
                op("dve", lambda e: e.reciprocal(out=st8[:, 0:8], in_=st8[:, 0:8]), w=["st8"], small=True)
                k3 = kkn[:].rearrange("p (h f) -> p h f", f=64)
                op("dve", lambda e: e.tensor_tensor(out=k3, in0=k3, in1=st8[:, 0:8].unsqueeze(2).to_broadcast([128, 8, 64]), op=ALU.mult),
                   r=["st8"], w=["kkn"], small=True)
                op("pool", lambda e: e.tensor_tensor(out=akk[:], in0=kkn[:], in1=aa[:], op=ALU.mult), r=["kkn", "aa"], w=["akk"])
                op("pool", lambda e: e.tensor_tensor(out=tB[:], in0=aa[:], in1=kar[:], op=ALU.mult), r=["aa", "rwc"], w=["tB"])
                op("pool", lambda e: e.tensor_tensor(out=tB[:], in0=tB[:], in1=omkar[:], op=ALU.add), r=["omkar"], w=["tB"])
                op("dve", lambda e: e.tensor_tensor(out=kw[:], in0=kr_, in1=tB[:], op=ALU.mult), r=krk + ["tB"], w=["kw"])
                if dog:
                    op("dve", lambda e: e.tensor_tensor(out=tA[:], in0=rr, in1=kr_, op=ALU.mult), r=krk, w=["tA"])
                    op("dve", lambda e: e.tensor_tensor(out=tA[:], in0=tA[:], in1=rkr[:], op=ALU.mult), r=["rwc"], w=["tA"])
                    op("dve", lambda e: e.tensor_reduce(out=st8[:, 8:16], in_=tA[:].rearrange("p (h f) -> p h f", f=64), axis=AX.X, op=ALU.add),
                       r=["tA"], w=["st8b"], small=True)
                    op("dve", lambda e: e.tensor_tensor(out=bon[:, j, :].rearrange("p (h f) -> p h f", f=64),
                                                        in0=v_.rearrange("p (h f) -> p h f", f=64),
                                                        in1=st8[:, 8:16].unsqueeze(2).to_broadcast([128, 8, 64]), op=ALU.mult),
                       r=["st8b", "p0"], w=["bon"], small=True)
                op("pe", lambda e: e.matmul(pb[3][:], lhsT=tri_f[:, mI, :], rhs=lw[:], start=True, stop=True), r=["lw", "tri_f"], w=[KB[3]])
                op("pe", lambda e: e.matmul(pb[4][:], lhsT=ones_f[:], rhs=lw[:], start=True, stop=True), r=["lw", "ones_f"], w=[KB[4]])
                for pr_ in range(4):
                    op("pe", lambda e, pr_=pr_: e.matmul(pb[5][:, pr_:pr_ + 1], lhsT=lw[:, pr_ * 128:(pr_ + 1) * 128], rhs=ones_f[:, 0:1],
                                                         start=True, stop=True), r=["lw", "ones_f"], w=[KB[5]], signal=(pr_ == 3))
                op("act", lambda e: e.activation(out=cum[:], in_=pb[3][:], func=AF.Copy), w=["cum", KB[3]])
                op("act", lambda e: e.activation(out=Eneg[:], in_=cum[:], func=AF.Exp, scale=-C), r=["cum"], w=["Eneg"])
                op("act", lambda e: e.activation(out=Epos[:], in_=cum[:], func=AF.Exp, scale=C), r=["cum"], w=["Epos"])
                op("pool", lambda e: e.tensor_tensor(out=Eex[:], in0=cum[:], in1=lw[:], op=ALU.subtract), r=["cum", "lw"], w=["Eex"])
                op("act", lambda e: e.activation(out=Eex[:], in_=Eex[:], func=AF.Exp, scale=-C), w=["Eex"])
                op("dve", lambda e: e.tensor_tensor(out=Ehat[:], in0=pb[4][:], in1=cum[:], op=ALU.subtract), r=["cum"], w=["Ehat", KB[4]])
                op("act", lambda e: e.activation(out=Ehat[:], in_=Ehat[:], func=AF.Exp, scale=-C), w=["Ehat"])
                op("act", lambda e: e.activation(out=PCf[:], in_=pb[5][:, 0:4], func=AF.Exp, scale=-C), w=["PCf", KB[5]], small=True)
                for idx, (eng, a_, b_) in enumerate((("dve", kkn, Eex), ("pool", rr, Eneg), ("dve", kw, Epos), ("pool", akk, Epos),
                                                      ("dve", kw, Ehat), ("pool", akk, Ehat))):
                    a_ap = a_ if not hasattr(a_, "ap") or idx == 1 else a_[:]
                    if idx == 1:
                        a_ap = rr
                    op(eng, lambda e, idx=idx, a_ap=a_ap, b_=b_: e.tensor_tensor(out=tok6[:, idx, :], in0=a_ap, in1=b_[:], op=ALU.mult),
                       r=krk + ["kkn", "kw", "akk", "Eex", "Eneg", "Epos", "Ehat"], w=[("tok6", idx)])
                if KRWL < 4:
                    continue
                for wi, src in enumerate((0, 3, 2, 1)):
                    if wi == 3 and not lat:
                        continue
                    ptt = pt[wi // 2]
                    for pr_ in range(4):
                        op("pe", lambda e, wi=wi, src=src, pr_=pr_, ptt=ptt: e.transpose(
                            out=ptt[:, (wi % 2) * 4 + pr_, :], in_=tok6[:, src, pr_ * 128:(pr_ + 1) * 128], identity=ident_b[:]),
                            r=[("tok6", src)], w=[KT[wi // 2]], signal=(pr_ == 3))
                op("act", lambda e: e.activation(out=fT[:, 0:2, :, :], in_=pt[0][:].rearrange("p (w a) t -> p w a t", w=2), func=AF.Copy),
                   w=["fT01", KT[0]])
                nw = 2 if lat else 1
                op("dve", lambda e: e.tensor_copy(out=fT[:, 2:2 + nw, :, :], in_=pt[1][:, 0:4 * nw, :].rearrange("p (w a) t -> p w a t", w=nw)),
                   w=["fT23", KT[1]])
                nprod = 5 if lat else 3
                plist = [(1, 0, mX, True), (0, 1, mY, True), (2, 0, mX, False), (1, 3, mI, False), (2, 3, mI, False)][:nprod]
                rounds = [plist[0:3]] + ([plist[3:5]] if lat else [])
                pbase = 0
                for rnd in rounds:
                    for idx, (li, ri, mk, neg) in enumerate(rnd):
                        for q_ in range(4):
                            for par in range(2):
                                hb = par * 64
                                bank = idx * 2 + par
                                op("pe", lambda e, li=li, ri=ri, q_=q_, hb=hb, bank=bank: e.matmul(
                                    pb[bank][:, q_ * 128:(q_ + 1) * 128], lhsT=fT[hb:hb + 64, li, q_, :], rhs=fT[hb:hb + 64, ri, q_, :],
                                    start=True, stop=True), r=["fT01", "fT23"], w=[KB[bank]], signal=(q_ == 3))
                    for idx, (li, ri, mk, neg) in enumerate(rnd):
                        pi = pbase + idx
                        msk = (ntri if neg else tri_f)[:, mk, :].unsqueeze(1).to_broadcast([128, 4, 128])
                        for par in range(2):
                            bank = idx * 2 + par
                            dst = prod[:, pi].rearrange("p (q two) t -> p q two t", two=2)[:, :, par, :]
                            op("dve", lambda e, dst=dst, msk=msk, bank=bank: e.tensor_tensor(
                                out=dst, in0=pb[bank][:].rearrange("p (h t) -> p h t", t=128), in1=msk, op=ALU.mult),
                                r=["ntri", "tri_f"], w=[("prod", pi), KB[bank]])
                    pbase += len(rnd)
                if KRWL < 5:
                    continue
                Xf, Yf = prod[:, 0], prod[:, 1]
                h3 = lambda ap: ap.bitcast(BF16).rearrange("p (h t) -> p h t", t=128)
                A0, B0 = XY2[:, 0], XY2[:, 1]
                A1, B1, Tn = h3(akk[:]), h3(kw[:]), h3(kkn[:])
                idb8 = ident_b[:, :].unsqueeze(1).to_broadcast([128, 8, 128])
                bd = blkb[:, 0, :].unsqueeze(1).to_broadcast([128, 8, 128])
                dead = [("tok6", q_) for q_ in range(6)]
                def hk(k_, hf):
                    return (k_, "hf", hf)

                def mm8(bank0, lh, rh, rk, nm):
                    for h in range(8):
                        hf = h // 4
                        op("pe", lambda e, h=h: e.matmul(pb[bank0 + h // 4][:, (h % 4) * 128:(h % 4 + 1) * 128], lhsT=lh[:, h, :], rhs=rh[:, h, :],
                                                         start=True, stop=True), r=[hk(k_, hf) for k_ in rk], w=[KB[bank0 + hf]], signal=(h % 4 == 3))

                def ev8(bank0, dst, kd, extra=()):
                    for hf in range(2):
                        op("act", lambda e, hf=hf: e.activation(out=dst[:, hf * 4:hf * 4 + 4, :], in_=pb[bank0 + hf][:].rearrange("p (h t) -> p h t", t=128),
                                                                func=AF.Copy), r=list(extra), w=[hk(kd, hf), KB[bank0 + hf]])

                def acc8(bank0, dst, kd):
                    for hf in range(2):
                        op("dve", lambda e, hf=hf: e.tensor_tensor(out=dst[:, hf * 4:hf * 4 + 4, :], in0=pb[bank0 + hf][:].rearrange("p (h t) -> p h t", t=128),
                                                                   in1=dst[:, hf * 4:hf * 4 + 4, :], op=ALU.add), w=[hk(kd, hf), KB[bank0 + hf]])

                def msk8(dst, kd, src, ksrc, m_, extra=()):
                    for hf in range(2):
                        op("pool", lambda e, hf=hf: e.tensor_tensor(out=dst[:, hf * 4:hf * 4 + 4, :], in0=src[:, hf * 4:hf * 4 + 4, :],
                                                                    in1=m_.unsqueeze(1).to_broadcast([128, 4, 128]), op=op_), r=[ksrc, "blkb", "ident_b"] + list(extra),
                           w=[hk(kd, hf)])

                op_ = ALU.mult
                msk8(A0, "XY2a", Xf, ("prod", 0), blkb[:, 0, :])
                msk8(B0, "XY2b", Yf, ("prod", 1), blkb[:, 0, :])
                op_ = ALU.add
                for hf in range(2):
                    op("pool", lambda e, hf=hf: e.tensor_tensor(out=Tt[:, hf * 4:hf * 4 + 4, :], in0=A0[:, hf * 4:hf * 4 + 4, :],
                                                                in1=ident_b[:, :].unsqueeze(1).to_broadcast([128, 4, 128]), op=ALU.add),
                       r=[hk("XY2a", hf), "ident_b", "Tt"], w=[hk("Tt", hf)])
                    op("pool", lambda e, hf=hf: e.tensor_tensor(out=Tn[:, hf * 4:hf * 4 + 4, :], in0=B0[:, hf * 4:hf * 4 + 4, :],
                                                                in1=ident_b[:, :].unsqueeze(1).to_broadcast([128, 4, 128]), op=ALU.add),
                       r=[hk("XY2b", hf), "ident_b", "kkn"] + dead, w=[hk("Tn", hf)])
                op_ = ALU.mult
                Xc, Yc, kX, kY = A0, B0, "XY2a", "XY2b"
                for lvl in range(3):
                    Xn, Yn, kXn, kYn = (A1, B1, "A1", "B1") if lvl % 2 == 0 else (A0, B0, "XY2a", "XY2b")
                    mm8(2, Xc, Yc, [kX, kY], "Yn")
                    mm8(0, Yc, Xc, [kX, kY], "Xn")
                    ev8(2, Yn, kYn, dead + ["akk", "kw"])
                    ev8(0, Xn, kXn, dead + ["akk", "kw"])
                    mm8(4, Yn, Tt[:], [kYn, "Tt"], "XnTt")
                    mm8(0, Xn, Tn, [kXn, "Tn"], "YnTn")
                    acc8(4, Tt[:], "Tt")
                    acc8(0, Tn, "Tn")
                    Xc, Yc, kX, kY = Xn, Yn, kXn, kYn
                for mk in range(3):
                    lastm = mk == 2
                    msk8(A0, "XY2a", Xf, ("prod", 0), blkb[:, 1 + mk, :])
                    msk8(B0, "XY2b", Yf, ("prod", 1), blkb[:, 1 + mk, :])
                    mm8(0, B0, Tt[:], ["XY2b", "Tt"], "M1")
                    if not lastm:
                        mm8(2, A0, Tn, ["XY2a", "Tn"], "M2")
                    ev8(0, A1, "A1")
                    if not lastm:
                        ev8(2, B1, "B1")
                    mm8(4, Tn, A1, ["Tn", "A1"], "TtM1")
                    if not lastm:
                        mm8(0, Tt[:], B1, ["Tt", "B1"], "TnM2")
                    acc8(4, Tt[:], "Tt")
                    if not lastm:
                        acc8(0, Tn, "Tn")
                op("pool", lambda e: e.memset(st8[:, 24:25], 0.0),
                   r=[hk(k_, hf) for k_ in ("Tt", "Tn", "A1", "B1", "XY2a", "XY2b") for hf in range(2)], w=["Tt", "kkn", "akk", "kw", ("XY2", 0), ("XY2", 1)])
                if KRWL < 6:
                    continue
                for h in range(8):
                    op("pe", lambda e, h=h: e.matmul(pb[0][:, h * 64:(h + 1) * 64], lhsT=prod[:, 2, h, :], rhs=vb[:, h * 64:(h + 1) * 64], start=True, stop=True),
                       r=[("prod", 2), "vb"], w=[KB[0]], signal=(h == 7))
                op("act", lambda e: e.activation(out=Zs[:], in_=pb[0][:], func=AF.Copy), w=["Zs", KB[0]])
                for h in range(8):
                    op("pe", lambda e, h=h: e.matmul(pb[1][:, h * 64:(h + 1) * 64], lhsT=Tt[:, h, :], rhs=Zs[:, h * 64:(h + 1) * 64], start=True, stop=True),
                       r=["Tt", "Zs"], w=[KB[1]], signal=(h == 7))
                op("act", lambda e: e.activation(out=nW[:], in_=pb[1][:], func=AF.Copy, scale=-1.0), w=["nW", KB[1]])
                for h in range(8):
                    op("pe", lambda e, h=h: e.matmul(pb[2][:, h * 64:(h + 1) * 64], lhsT=Tt[:, h, :], rhs=tok6[:, 0, h * 64:(h + 1) * 64], start=True, stop=True),
                       r=["Tt", ("tok6", 0)], w=[KB[2]], signal=(h == 7))
                op("act", lambda e: e.activation(out=Abs_[:], in_=pb[2][:], func=AF.Copy), w=["Abs", KB[2]])
                for h in range(8):
                    pair, hb = h // 2, (h % 2) * 64
                    op("pe", lambda e, h=h, pair=pair, hb=hb: e.matmul(pb[4][hb:hb + 64, pair * 64:(pair + 1) * 64], lhsT=Abs_[:, h * 64:(h + 1) * 64],
                                                                       rhs=tok6[:, 5, h * 64:(h + 1) * 64], start=True, stop=True, tile_position=(0, hb)),
                       r=["Abs", ("tok6", 5)], w=[KB[4]], signal=(h == 7))
                for pair in range(4):
                    op("dve", lambda e, pair=pair: e.scalar_tensor_tensor(out=Gt[:, pair, :], in0=idp[:], scalar=PCf[:, pair:pair + 1],
                                                                          in1=pb[4][:, pair * 64:(pair + 1) * 64], op0=ALU.mult, op1=ALU.subtract),
                       r=["idp", "PCf"], w=["Gt", KB[4]], small=True)
                if KRWL < 7:
                    continue
                if lat:
                    for h in range(8):
                        pair, hb = h // 2, (h % 2) * 64
                        op("pe", lambda e, h=h, pair=pair, hb=hb: e.matmul(pb[3][hb:hb + 64, pair * 128:(pair + 1) * 128], lhsT=Abs_[:, h * 64:(h + 1) * 64],
                                                                           rhs=prod[:, 3, h, :], start=True, stop=True, tile_position=(0, hb)),
                           r=["Abs", ("prod", 3)], w=[KB[3]], signal=(h == 7))
                    op("dve", lambda e: e.tensor_tensor(out=RbT[:], in0=fT[:, 3, :, :], in1=pb[3][:].rearrange("p (a t) -> p a t", t=128), op=ALU.subtract),
                       r=["fT23"], w=["RbT", KB[3]])
                    for h in range(8):
                        pair, hb = h // 2, (h % 2) * 64
                        o_ = pb[5][:, h * 64:(h + 1) * 64]
                        op("pe", lambda e, h=h, o_=o_: e.matmul(o_, lhsT=prod[:, 4, h, :], rhs=vb[:, h * 64:(h + 1) * 64], start=True, stop=False),
                           r=[("prod", 4), "vb"], w=[KB[5]], signal=False)
                        op("pe", lambda e, h=h, o_=o_: e.matmul(o_, lhsT=prod[:, 3, h, :], rhs=nW[:, h * 64:(h + 1) * 64], start=False, stop=False),
                           r=[("prod", 3), "nW"], w=[KB[5]], signal=False)
                        op("pe", lambda e, h=h, o_=o_, pair=pair, hb=hb: e.matmul(o_, lhsT=RbT[hb:hb + 64, pair, :], rhs=H[hb:hb + 64, pair, :],
                                                                                 start=False, stop=True), r=["RbT", kH], w=[KB[5]], signal=(h == 7))
                    op("dve", lambda e: e.tensor_tensor(out=oacc[:, j, :], in0=pb[5][:], in1=oacc[:, j, :], op=ALU.add), w=["oacc", KB[5]])
                for h in range(8):
                    pair, hb = h // 2, (h % 2) * 64
                    o_ = pb[0][hb:hb + 64, pair * 64:(pair + 1) * 64]
                    op("pe", lambda e, h=h, o_=o_, hb=hb: e.matmul(o_, lhsT=tok6[:, 4, h * 64:(h + 1) * 64], rhs=vb[:, h * 64:(h + 1) * 64], start=True, stop=False,
                                                                   tile_position=(0, hb)), r=[("tok6", 4), "vb"], w=[KB[0]], signal=False)
                    op("pe", lambda e, h=h, o_=o_, hb=hb: e.matmul(o_, lhsT=tok6[:, 5, h * 64:(h + 1) * 64], rhs=nW[:, h * 64:(h + 1) * 64], start=False, stop=False,
                                                                   tile_position=(0, hb)), r=[("tok6", 5), "nW"], w=[KB[0]], signal=False)
                    op("pe", lambda e, h=h, o_=o_, pair=pair, hb=hb: e.matmul(o_, lhsT=Gt[hb:hb + 64, pair, :], rhs=H[hb:hb + 64, pair, :], start=False, stop=True,
                                                                             tile_position=(hb, hb)), r=["Gt", kH], w=[KB[0]], signal=(h == 7))
                op("act", lambda e, H=H: e.activation(out=H[:], in_=pb[0][:, 0:256].rearrange("p (a v) -> p a v", v=64), func=AF.Copy), w=[kH, KB[0]])
        if dbg and "oacc" in dbg and b == 0 and nvis >= NT:
            S.dma("sp", dbg_d["oacc"][:, :, :], oacc[:], r=["oacc"], chan="dbg")
        nfin = 16 if nvis >= NT else 0
        for j in range(nfin):
            o3 = oacc[:, j, :].rearrange("p (h f) -> p h f", f=64)
            op("dve", lambda e: e.tensor_reduce(out=st8[:, 0:8], in_=o3, axis=AX.X, op=ALU.add), r=["oacc"], w=["st8"], small=True)
            op("pool", lambda e: e.tensor_tensor(out=tA[:], in0=oacc[:, j, :], in1=oacc[:, j, :], op=ALU.mult), r=["oacc"], w=["tA"])
            op("dve", lambda e: e.tensor_reduce(out=st8[:, 8:16], in_=tA[:].rearrange("p (h f) -> p h f", f=64), axis=AX.X, op=ALU.add),
               r=["tA"], w=["st8b"], small=True)
            op("dve", lambda e: e.tensor_scalar(out=st8[:, 0:8], in0=st8[:, 0:8], scalar1=1.0 / 64, scalar2=None, op0=ALU.mult), w=["st8"], small=True)
            op("dve", lambda e: e.tensor_tensor(out=st8[:, 16:24], in0=st8[:, 0:8], in1=st8[:, 0:8], op=ALU.mult), r=["st8"], w=["st8c"], small=True)
            op("dve", lambda e: e.scalar_tensor_tensor(out=st8[:, 8:16], in0=st8[:, 8:16], scalar=1.0 / 64, in1=st8[:, 16:24], op0=ALU.mult, op1=ALU.subtract),
               r=["st8c"], w=["st8b"], small=True)
            op("dve", lambda e: e.tensor_scalar(out=st8[:, 8:16], in0=st8[:, 8:16], scalar1=64e-5, scalar2=None, op0=ALU.add), w=["st8b"], small=True)
            op("act", lambda e: e.activation(out=st8[:, 8:16], in_=st8[:, 8:16], func=AF.Sqrt), w=["st8b"], small=True)
            op("dve", lambda e: e.reciprocal(out=st8[:, 8:16], in_=st8[:, 8:16]), w=["st8b"], small=True)
            a3 = tA[:].rearrange("p (h f) -> p h f", f=64)
            op("dve", lambda e: e.tensor_tensor(out=a3, in0=o3, in1=st8[:, 0:8].unsqueeze(2).to_broadcast([128, 8, 64]), op=ALU.subtract),
               r=["oacc", "st8"], w=["tA"], small=True)
            op("dve", lambda e: e.tensor_tensor(out=a3, in0=a3, in1=st8[:, 8:16].unsqueeze(2).to_broadcast([128, 8, 64]), op=ALU.mult),
               r=["st8b"], w=["tA"], small=True)
            op("pool", lambda e: e.tensor_tensor(out=tA[:], in0=tA[:], in1=lngr[:], op=ALU.mult), r=["rwc"], w=["tA"])
            op("pool", lambda e: e.tensor_tensor(out=tA[:], in0=tA[:], in1=lnbr[:], op=ALU.add), r=["rwc"], w=["tA"])
            op("dve", lambda e: e.tensor_tensor(out=tA[:], in0=tA[:], in1=bon[:, j, :], op=ALU.add), r=["bon"], w=["tA"])
            op("pe", lambda e, j=j: e.transpose(out=pt[1][:, 0, :], in_=sgd[:, j, :], identity=ident_b[:]), r=["sgd"], w=[KT[1]])
            op("act", lambda e: e.activation(out=lT[:, 2, :], in_=pt[1][:, 0, :], func=AF.Copy), w=["lT", KT[1]])
            op("pe", lambda e: e.matmul(pb[2][:], lhsT=lT[:, 2, :], rhs=gup[:], start=True, stop=True), r=["lT", "rwc2"], w=[KB[2]])
            op("dve", lambda e: e.tensor_tensor(out=rwtok[:], in0=pb[2][:], in1=tA[:], op=ALU.mult), r=["tA"], w=["rwtok", KB[2]])
            for pr_ in range(4):
                op("pe", lambda e, pr_=pr_: e.transpose(out=pt[0][:, pr_, :], in_=rwtok[:, pr_ * 128:(pr_ + 1) * 128], identity=ident_b[:]),
                   r=["rwtok"], w=[KT[0]], signal=(pr_ == 3))
            op("act", lambda e, j=j: e.activation(out=mixT[:, 4:8, j * 128:(j + 1) * 128], in_=pt[0][:, 0:4, :], func=AF.Copy), w=["mixT_rw", KT[0]])
        if dbg and "rwT" in dbg and b == 0 and nvis >= NT:
            S.dma("sp", dbg_d["rwT"][:, :, :], mixT[:, 4:8, :], r=["mixT_rw"], chan="dbg")
        S.barrier()


def gate_row(S, sb, ps, st, G, b, m, name):
    modT, ident_f = G["modT"], G["ident_f"]
    row = sb(name, [128, D], stack=st)
    with contextlib.ExitStack() as s2:
        tmpb = sb("tmpb", [128, 128], stack=s2)
        ps_g = [ps("ps_g%d" % i, [128, 512], stack=s2) for i in range(2)]
        for j in range(8):
            S.op("dve", lambda e, j=j: e.tensor_copy(out=tmpb[:], in_=modT[:, m * 8 + j, b:b + 1].to_broadcast([128, 128])),
                 r=["modT"], w=["tmpb"])
            S.op("pe", lambda e, j=j: e.matmul(ps_g[j // 4][:, (j % 4) * 128:(j % 4 + 1) * 128], lhsT=tmpb[:], rhs=ident_f[:], start=True, stop=True),
                 r=["tmpb", "ident_f"], w=[("ps_g", j // 4)])
        for hf in range(2):
            S.op("dve", lambda e, hf=hf: e.tensor_copy(out=row[:, hf * 512:(hf + 1) * 512], in_=ps_g[hf][:]), w=[name, ("ps_g", hf)])
        S.barrier()
    return row


def stage_O(nc, S, sb, ps, b, G, dbg, dbg_d, mixT, hx2T, comb, rms_rstd):
    ident_f, epsc = G["ident_f"], G["epsc"]
    modT, gsT = G["modT"], G["gsT"]
    x_d, y_d = G["x_d"], G["y_d"]
    with contextlib.ExitStack() as st:
        g2row = gate_row(S, sb, ps, st, G, b, 2, "g2row")
        w_out = sb("w_out", [128, 8, D], BF16, stack=st)
        wcat = sb("wcat", [128, 8, 36], stack=st)
        bcat = sb("bcat", [128, 36], stack=st)
        xt = [sb("xto%d" % i, [128, D], stack=st) for i in range(2)]
        xm = sb("xm", [128, D], stack=st)
        junk = sb("junko", [128, D], BF16, stack=st)
        hf32 = sb("hf32", [128, 8, 128], stack=st)
        rstd = sb("rstdo", [128, 2], stack=st)
        rt = sb("rt", [128, 96], stack=st)
        ps_y = [ps("ps_y%d" % i, [128, 512], stack=st) for i in range(2)]
        ps_tr = [ps("ps_tr%d" % i, [128, 4, 128], stack=st) for i in range(2)]
        ps_l = ps("ps_lg", [128, 64], stack=st)
        S.dma("pool", w_out[:], G["wout_d"].rearrange("(kc p) n -> p kc n", p=128), w=["w_out"])
        S.dma("sp", wcat[:], G["wcat_d"].rearrange("(kc p) n -> p kc n", p=128), w=["wcat"])
        S.dma("sp", bcat[:], G["bcat_d"].partition_broadcast(128), w=["bcat"])
        for j in range(16):
            xb_ = xt[j % 2]
            kx = ("xto", j % 2)
            S.dma("sp", xb_[:], x_d[b, j * 128:(j + 1) * 128, :], w=[kx])
            for n in range(2):
                for kc in range(8):
                    S.op("pe", lambda e, n=n, kc=kc: e.matmul(ps_y[n][:], lhsT=mixT[:, kc, j * 128:(j + 1) * 128], rhs=w_out[:, kc, n * 512:(n + 1) * 512],
                                                              start=(kc == 0), stop=(kc == 7)), r=["mixT_na", "mixT_rw", "w_out"], w=[("ps_y", n)], signal=(kc == 7))
            for n in range(2):
                S.op("dve", lambda e, n=n: e.tensor_tensor(out=xm[:, n * 512:(n + 1) * 512], in0=ps_y[n][:], in1=g2row[:, n * 512:(n + 1) * 512], op=ALU.mult),
                     r=["g2row"], w=[("xm", n), ("ps_y", n)])
                S.op("pool", lambda e, n=n: e.tensor_tensor(out=xm[:, n * 512:(n + 1) * 512], in0=xm[:, n * 512:(n + 1) * 512], in1=xb_[:, n * 512:(n + 1) * 512], op=ALU.add),
                     r=[kx], w=[("xm", n)])
            S.dma("sp", y_d[b, j * 128:(j + 1) * 128, :], xm[:], r=[("xm", 0), ("xm", 1)], w=[("ymid", j)], chan=("yw", j % 2))
            rs = rstd[:, 0:1]
            S.op("act", lambda e: e.activation(out=junk[:], in_=xm[:], func=AF.Square, accum_out=rs), r=[("xm", 0), ("xm", 1)], w=["rstdo", "junko"])
            S.op("act", lambda e: e.activation(out=rs, in_=rs, func=AF.Sqrt, bias=epsc[:, 0:1], scale=1.0 / D), r=["epsc"], w=["rstdo"])
            S.op("dve", lambda e: e.reciprocal(out=rs, in_=rs), w=["rstdo"])
            S.op("dve", lambda e: e.tensor_scalar(out=xb_[:], in0=xm[:], scalar1=rs, scalar2=None, op0=ALU.mult), r=[("xm", 0), ("xm", 1), "rstdo"], w=[kx])
            for kc in range(8):
                S.op("pe", lambda e, kc=kc: e.transpose(out=ps_tr[kc // 4][:, kc % 4, :], in_=xb_[:, kc * 128:(kc + 1) * 128], identity=ident_f[:]),
                     r=[kx, "ident_f"], w=[("ps_tr", kc // 4)], signal=(kc % 4 == 3))
            for kc in range(8):
                S.op("dve" if kc < 4 else "act", (lambda e, kc=kc: e.tensor_scalar(out=hf32[:, kc, :], in0=ps_tr[kc // 4][:, kc % 4, :], scalar1=gsT[:, 1, kc, b:b + 1],
                                                                                     scalar2=modT[:, 24 + kc, b:b + 1], op0=ALU.mult, op1=ALU.add)) if kc < 4 else
                     (lambda e, kc=kc: e.activation(out=hf32[:, kc, :], in_=ps_tr[kc // 4][:, kc % 4, :], func=AF.Identity, bias=modT[:, 24 + kc, b:b + 1],
                                                    scale=gsT[:, 1, kc, b:b + 1])), r=["modT", "gsT"], w=[("hf32", kc), ("ps_tr", kc // 4)])
            S.op("pool", lambda e, j=j: e.tensor_copy(out=hx2T[:, :, j * 128:(j + 1) * 128], in_=hf32[:]), r=[("hf32", k_) for k_ in range(8)], w=["hx2T", "mixT_na", "mixT_rw"])
            for kc in range(8):
                S.op("pe", lambda e, kc=kc: e.matmul(ps_l[:, 0:36], lhsT=hf32[:, kc, :], rhs=wcat[:, kc, :], start=(kc == 0), stop=(kc == 7)),
                     r=[("hf32", kc), "wcat"], w=["ps_l"], signal=(kc == 7))
            lg, le = rt[:, 0:4], rt[:, 4:36]
            sm_ = lambda fn, r=(), w=("rt",), eng="dve": S.op(eng, fn, r=list(r), w=list(w), small=True)
            sm_(lambda e: e.tensor_tensor(out=rt[:, 0:36], in0=ps_l[:, 0:36], in1=bcat[:], op=ALU.add), r=["bcat"], w=["rt", "ps_l"])
            gmax, ngmax, se, pg = rt[:, 36:37], rt[:, 37:38], rt[:, 38:39], rt[:, 39:40]
            goh, esel = rt[:, 40:44], rt[:, 44:52]
            m1, m2, oh1, oh2 = rt[:, 52:53], rt[:, 53:54], rt[:, 54:62], rt[:, 62:70]
            e2, dd, w1, w2 = rt[:, 70:78], rt[:, 78:79], rt[:, 79:80], rt[:, 80:81]
            cw, tmp4 = rt[:, 81:89], rt[:, 89:93]
            sm_(lambda e: e.tensor_reduce(out=gmax, in_=lg, axis=AX.X, op=ALU.max))
            sm_(lambda e: e.tensor_scalar(out=ngmax, in0=gmax, scalar1=-1.0, scalar2=None, op0=ALU.mult))
            sm_(lambda e: e.tensor_scalar(out=goh, in0=lg, scalar1=gmax, scalar2=None, op0=ALU.is_equal))
            sm_(lambda e: e.activation(out=tmp4, in_=lg, func=AF.Exp, bias=ngmax, scale=1.0, accum_out=se), eng="act")
            sm_(lambda e: e.reciprocal(out=pg, in_=se))
            big = G["big32"]
            sm_(lambda e: e.tensor_tensor(out=big[:, 0:32].rearrange("p (g x) -> p g x", x=8), in0=le.rearrange("p (g x) -> p g x", x=8),
                                          in1=goh.unsqueeze(2).to_broadcast([128, 4, 8]), op=ALU.mult), w=["rt", "big32"])
            sm_(lambda e: e.tensor_reduce(out=esel, in_=big[:, 0:32].rearrange("p (g x) -> p x g", x=8), axis=AX.X, op=ALU.add), w=["rt", "big32"])
            sm_(lambda e: e.tensor_reduce(out=m1, in_=esel, axis=AX.X, op=ALU.max))
            sm_(lambda e: e.tensor_scalar(out=oh1, in0=esel, scalar1=m1, scalar2=None, op0=ALU.is_equal))
            sm_(lambda e: e.scalar_tensor_tensor(out=e2, in0=oh1, scalar=-1e30, in1=esel, op0=ALU.mult, op1=ALU.add))
            sm_(lambda e: e.tensor_reduce(out=m2, in_=e2, axis=AX.X, op=ALU.max))
            sm_(lambda e: e.tensor_scalar(out=oh2, in0=e2, scalar1=m2, scalar2=None, op0=ALU.is_equal))
            sm_(lambda e: e.tensor_tensor(out=dd, in0=m2, in1=m1, op=ALU.subtract))
            sm_(lambda e: e.activation(out=dd, in_=dd, func=AF.Exp), eng="act")
            sm_(lambda e: e.tensor_scalar(out=w1, in0=dd, scalar1=1.0, scalar2=None, op0=ALU.add))
            sm_(lambda e: e.reciprocal(out=w1, in_=w1))
            sm_(lambda e: e.tensor_tensor(out=w2, in0=dd, in1=w1, op=ALU.mult))
            sm_(lambda e: e.tensor_tensor(out=w1, in0=w1, in1=pg, op=ALU.mult))
            sm_(lambda e: e.tensor_tensor(out=w2, in0=w2, in1=pg, op=ALU.mult))
            sm_(lambda e: e.tensor_scalar(out=cw, in0=oh1, scalar1=w1, scalar2=None, op0=ALU.mult))
            sm_(lambda e: e.scalar_tensor_tensor(out=cw, in0=oh2, scalar=w2, in1=cw, op0=ALU.mult, op1=ALU.add))
            sm_(lambda e, j=j: e.tensor_tensor(out=comb[:, j, :].rearrange("p (g x) -> p g x", x=8), in0=goh.unsqueeze(2).to_broadcast([128, 4, 8]),
                                               in1=cw.unsqueeze(1).to_broadcast([128, 4, 8]), op=ALU.mult), w=["rt", "comb"])
        if dbg and "comb" in dbg and b == 0:
            S.dma("sp", dbg_d["comb"][:, :, :], comb[:], r=["comb"], chan="dbg")
        S.barrier()


def stage_MOE(nc, S, sb, ps, b, G, dbg, dbg_d, hx2T, comb):
    import os
    y_d = G["y_d"]
    with contextlib.ExitStack() as st:
        g5row = gate_row(S, sb, ps, st, G, b, 5, "g5row")
        acc = sb("acc", [128, 16, D], stack=st)
        w13 = [sb("w13_%d" % i, [128, 2, 8, 512], BF16, stack=st) for i in range(2)]
        w2s = [sb("w2s_%d" % i, [128, 4, D], BF16, stack=st) for i in range(2)]
        he = [sb("he%d" % i, [128, 4, 512], BF16, stack=st) for i in range(2)]
        sa = [sb("sa%d" % i, [128, 512], stack=st) for i in range(2)]
        xr = [sb("xr%d" % i, [128, D], stack=st) for i in range(2)]
        ps_a = [ps("ps_a%d" % i, [128, 512], stack=st) for i in range(2)]
        ps_b = [ps("ps_b%d" % i, [128, 512], stack=st) for i in range(2)]
        ps_o = [ps("ps_o%d" % i, [128, 512], stack=st) for i in range(4)]
        S.op("pool", lambda e: e.memset(acc[:], 0.0), w=[("acc", t_) for t_ in range(16)])
        nexp = int(os.environ.get("KEXP", 32))
        cnt = {"ab": 0, "o": 0}

        def emit_ab(ex, tg):
            wb = ex % 2
            if tg == 0:
                S.dma("pool", w13[wb][:, 0], G["w1_d"][ex].rearrange("(kc p) n -> p kc n", p=128), w=[("w1", wb)])
                S.dma("pool", w13[wb][:, 1], G["w3_d"][ex].rearrange("(kc p) n -> p kc n", p=128), w=[("w3", wb)])
                S.dma("pool", w2s[wb][:], G["w2_d"][ex].rearrange("(kc p) n -> p kc n", p=128), w=[("w2", wb)])
            hb_ = he[tg % 2]
            khe = ("he", tg % 2)
            for fc in range(4):
                ab = cnt["ab"] % 2
                cnt["ab"] += 1
                for which, pss, kname in ((0, ps_a, "ps_a"), (1, ps_b, "ps_b")):
                    for kc in range(8):
                        S.op("pe", lambda e, which=which, pss=pss, kc=kc, fc=fc, ab=ab: e.matmul(
                            pss[ab][:], lhsT=w13[wb][:, which, kc, fc * 128:(fc + 1) * 128], rhs=hx2T[:, kc, tg * 512:(tg + 1) * 512],
                            start=(kc == 0), stop=(kc == 7)), r=[("w1", wb), ("w3", wb), "hx2T"], w=[(kname, ab)], signal=(kc == 7))
                S.op("act", lambda e, ab=ab: e.activation(out=sa[ab][:], in_=ps_a[ab][:], func=AF.Silu), w=[("sa", ab), ("ps_a", ab)])
                S.op("dve", lambda e, ab=ab, fc=fc, hb_=hb_: e.tensor_tensor(out=hb_[:, fc, :], in0=ps_b[ab][:], in1=sa[ab][:], op=ALU.mult),
                     r=[("sa", ab)], w=[khe, ("ps_b", ab)])

        def emit_w2(ex, tg):
            wb = ex % 2
            hb_ = he[tg % 2]
            khe = ("he", tg % 2)
            for tt in range(4):
                tile = tg * 4 + tt
                for n in range(2):
                    ob = cnt["o"] % 4
                    cnt["o"] += 1
                    for fc in range(4):
                        S.op("pe", lambda e, fc=fc, n=n, ob=ob, tt=tt: e.matmul(
                            ps_o[ob][:], lhsT=hb_[:, fc, tt * 128:(tt + 1) * 128], rhs=w2s[wb][:, fc, n * 512:(n + 1) * 512],
                            start=(fc == 0), stop=(fc == 3)), r=[khe, ("w2", wb)], w=[("ps_o", ob)], signal=(fc == 3))
                    S.op("dve", lambda e, n=n, ob=ob, tile=tile: e.scalar_tensor_tensor(
                        out=acc[:, tile, n * 512:(n + 1) * 512], in0=ps_o[ob][:], scalar=comb[:, tile, ex:ex + 1],
                        in1=acc[:, tile, n * 512:(n + 1) * 512], op0=ALU.mult, op1=ALU.add), r=["comb"], w=[("acc", tile), ("ps_o", ob)])

        items = [(ex, tg) for ex in range(nexp) for tg in range(4)]
        emit_ab(*items[0])
        for k_ in range(len(items)):
            if k_ + 1 < len(items):
                emit_ab(*items[k_ + 1])
            emit_w2(*items[k_])
        for j in range(16):
            xb_ = xr[j % 2]
            kx = ("xr", j % 2)
            S.dma("sp", xb_[:], y_d[b, j * 128:(j + 1) * 128, :], r=[("ymid", j)], w=[kx])
            S.op("pool", lambda e, j=j: e.tensor_tensor(out=acc[:, j, :], in0=acc[:, j, :], in1=g5row[:], op=ALU.mult), r=["g5row"], w=[("acc", j)])
            S.op("pool", lambda e, j=j: e.tensor_tensor(out=xb_[:], in0=xb_[:], in1=acc[:, j, :], op=ALU.add), r=[("acc", j)], w=[kx])
            S.dma("sp", y_d[b, j * 128:(j + 1) * 128, :], xb_[:], r=[kx], w=[("yfin", j)], chan=("yw", j % 2))
        S.barrier()
```

```python
import contextlib
import numpy as np
import concourse.bass as bass
import concourse.mybir as mybir
from concourse.bass_utils import run_bass_kernel_spmd

F32 = mybir.dt.float32
BF16 = mybir.dt.bfloat16
AF = mybir.ActivationFunctionType
ALU = mybir.AluOpType
AX = mybir.AxisListType

D = 1024
SEQ = 2048
CTX = 256
NB = 2
NT = 18
DIN = 3456
DRW = 1920
NEGM = -30000.0
CDEC = 0.6065306597126334
PS_ROWS = 2308
CTX_BASE = 1
LAT_BASE = 259


class Sched:
    def __init__(self, nc, es):
        self.nc = nc
        self.es = es
        self.eng = {"pe": nc.tensor, "act": nc.scalar, "dve": nc.vector, "pool": nc.gpsimd, "sp": nc.sync}
        self.sem = {}
        self.cnt = {}
        for e in ("pe", "act", "dve", "pool"):
            self.sem[e] = es.enter_context(nc.semaphore("s_" + e))
            self.cnt[e] = 0
        self.waited = {}
        self.last_w = {}
        self.reads = {}
        self.nops = 0

    def _src_sem(self, src):
        if src not in self.sem:
            self.sem[src] = self.es.enter_context(self.nc.semaphore("s_dma%d" % len(self.sem)))
            self.cnt[src] = 0
        return self.sem[src]

    def _wait(self, eng, deps, small):
        need = {}
        for (src, c, sm) in deps:
            if src == eng:
                if eng == "pe" or eng == "sp":
                    continue
            if need.get(src, 0) < c:
                need[src] = c
        for src, c in need.items():
            if self.waited.get((eng, src), 0) >= c:
                continue
            self.eng[eng].wait_ge(self._src_sem(src), c)
            self.waited[(eng, src)] = c

    def _deps(self, r, w):
        deps = []
        for k in r:
            if k in self.last_w:
                deps.append(self.last_w[k])
        for k in w:
            if k in self.last_w:
                deps.append(self.last_w[k])
            deps.extend(self.reads.get(k, ()))
        return deps

    def _record(self, me, r, w):
        for k in r:
            self.reads.setdefault(k, []).append(me)
        for k in w:
            self.last_w[k] = me
            self.reads[k] = []

    def op(self, eng, fn, r=(), w=(), small=False, signal=True):
        self._wait(eng, self._deps(r, w), small)
        inst = fn(self.eng[eng])
        if signal:
            inst.then_inc(self.sem[eng], 1)
            self.cnt[eng] += 1
            me = (eng, self.cnt[eng], small)
        else:
            me = (eng, self.cnt[eng] + 1, small)
        self._record(me, r, w)
        self.nops += 1
        return inst

    def dma(self, q, out, in_, r=(), w=(), chan=None):
        src = ("dma", chan if chan is not None else (w[0] if w else r[0]))
        sem = self._src_sem(src)
        deps = self._deps(r, w)
        if self.cnt[src] > 0:
            deps.append((src, self.cnt[src], False))
        self._wait(q, deps, False)
        self.eng[q].dma_start(out=out, in_=in_).then_inc(sem, 16)
        self.cnt[src] += 16
        me = (src, self.cnt[src], False)
        self._record(me, r, w)
        self.nops += 1

    def barrier(self):
        snap = [(src, c) for src, c in self.cnt.items() if c > 0]
        for e in ("pe", "act", "dve", "pool", "sp"):
            for src, c in snap:
                if src == e:
                    continue
                if self.waited.get((e, src), 0) >= c:
                    continue
                self.eng[e].wait_ge(self._src_sem(src), c)
                self.waited[(e, src)] = c

    def wait_all(self, eng="sp"):
        for src, c in self.cnt.items():
            if c > 0 and src != eng:
                self.eng[eng].wait_ge(self._src_sem(src), c)


def build(dbg=None, stages=("P", "NA", "RW", "O", "MOE"), nb=NB):
    nc = bass.Bass("TRN2", target_bir_lowering=False)

    declared = []

    def din(name, shape, dt=F32):
        declared.append(name)
        return nc.dram_tensor(name, list(shape), dt, kind="ExternalInput").ap()

    x_d = din("x", [NB, SEQ, D])
    ctx_d = din("ctx", [NB, CTX, D])
    cT_d = din("cT", [128, 8, 3])
    wmod_d = din("w_mod", [D, 6 * D])
    bmodT_d = din("bmodT", [128, 48])
    g1T_d = din("g1T", [128, 8])
    g2T_d = din("g2T", [128, 8])
    win_d = din("w_in", [D, DIN])
    qg_d = din("na_q_g", [64])
    kg_d = din("na_k_g", [64])
    mtab_d = din("mtab", [128, 32, 480])
    rmask_d = din("rmask", [2, 32 * 128])
    sel2_d = din("sel2", [2, 128])
    rope_d = din("rope", [128, 16, 2, 32])
    tri_d = din("tri", [128, 4, 128])
    ident_d = din("ident", [128, 128])
    blk_d = din("blk", [128, 4, 128])
    mup_d = din("rw_mu_prev", [DRW])
    mun_d = din("rw_mu_next", [DRW])
    w0_d = din("rw_w0", [2 * 512])
    wup_d = din("rw_w_up", [2, 64, 512])
    a0_d = din("rw_a0", [2 * 512])
    aup_d = din("rw_a_up", [2, 64, 512])
    gup_d = din("rw_g_up", [128, 512])
    kk_d = din("rw_k_k", [512])
    ka_d = din("rw_k_a", [512])
    rk_d = din("rw_r_k", [512])
    lng_d = din("rw_ln_g", [512])
    lnb_d = din("rw_ln_b", [512])
    wout_d = din("w_out", [D, D])
    wcat_d = din("wcat", [D, 36])
    bcat_d = din("bcat", [36])
    if "MOE" in stages:
        w1_d = din("moe_w1", [32, D, 512])
        w3_d = din("moe_w3", [32, D, 512])
        w2_d = din("moe_w2", [32, 512, D])
    y_d = nc.dram_tensor("y", [NB, SEQ, D], F32, kind="ExternalOutput").ap()
    pscr_d = nc.dram_tensor("pscr", [NB, PS_ROWS, DRW], F32, kind="Internal").ap()
    dbg_d = {}
    if dbg:
        for name, shape in dbg.items():
            dt_ = F32
            if shape[0] == "bf16":
                dt_, shape = BF16, shape[1:]
            dbg_d[name] = nc.dram_tensor("dbg_" + name, list(shape), dt_, kind="ExternalOutput").ap()

    with contextlib.ExitStack() as es:
        S = Sched(nc, es)

        uid = [0]

        def sb(name, shape, dt=F32, stack=es):
            uid[0] += 1
            return stack.enter_context(nc.sbuf_tensor("s%d_%s" % (uid[0], name), list(shape), dt))

        def ps(name, shape, dt=F32, stack=es):
            uid[0] += 1
            shape = list(shape)
            esz = 4 if dt == F32 else 2
            per = 1
            for d_ in shape[1:]:
                per *= d_
            inner = 1
            for d_ in shape[2:]:
                inner *= d_
            orig1 = shape[1]
            while (per * esz) % 2048 != 0:
                shape[1] += 1
                per = shape[1] * inner
            t_ = stack.enter_context(nc.psum_tensor("p%d_%s" % (uid[0], name), shape, dt))
            if shape[1] == orig1:
                return t_
            return t_[:][:, 0:orig1]

        ident_f = sb("ident_f", [128, 128])
        ident_b = sb("ident_b", [128, 128], BF16)
        tri_f = sb("tri_f", [128, 4, 128])
        ones_b = sb("ones_b", [128, 128], BF16)
        ones_f = sb("ones_f", [128, 128])
        modT = sb("modT", [128, 48, 3])
        gsT = sb("gsT", [128, 2, 8, 3])
        g1T = sb("g1T", [128, 8])
        g2T = sb("g2T", [128, 8])
        epsc = sb("epsc", [128, 1])
        S.dma("sp", ident_f[:], ident_d[:, :], w=["ident_f"])
        S.dma("sp", tri_f[:], tri_d[:, :, :], w=["tri_f"])
        S.dma("sp", g1T[:], g1T_d[:, :], w=["g1T"])
        S.dma("sp", g2T[:], g2T_d[:, :], w=["g2T"])
        S.op("dve", lambda e: e.tensor_copy(out=ident_b[:], in_=ident_f[:]), r=["ident_f"], w=["ident_b"])
        S.op("dve", lambda e: e.memset(ones_b[:], 1.0), w=["ones_b"])
        S.op("dve", lambda e: e.memset(ones_f[:], 1.0), w=["ones_f"])
        S.op("dve", lambda e: e.memset(epsc[:], 1e-6), w=["epsc"], small=True)

        import os
        KPRE = int(os.environ.get('KPRE', 9))
        with contextlib.ExitStack() as st:
            cT = sb("cT", [128, 8, 3], stack=st)
            bmT = sb("bmT", [128, 48], stack=st)
            slab = [sb("slab%d" % i, [128, 8, 1024], stack=st) for i in range(2)]
            ps_mod = ps("ps_mod", [128, 48, 4], stack=st)
            S.dma("sp", cT[:], cT_d[:, :, :], w=["cT"])
            S.dma("sp", bmT[:], bmodT_d[:, :], w=["bmT"])
            S.op("act", lambda e: e.activation(out=cT[:], in_=cT[:], func=AF.Silu), r=["cT"], w=["cT"], small=True)
            wv = wmod_d.rearrange("(kc p) (s f) -> s p kc f", p=128, f=1024)
            for s in range(6 if KPRE >= 1 else 0):
                sl = slab[s % 2]
                S.dma("sp" if s % 2 == 0 else "act", sl[:], wv[s], w=[("slab", s % 2)])
                for f in range(8):
                    for kc in range(8):
                        S.op("pe", lambda e, f=f, kc=kc, sl=sl, s=s: e.matmul(
                            ps_mod[:, s * 8 + f, 0:3], lhsT=sl[:, kc, f * 128:(f + 1) * 128], rhs=cT[:, kc, :],
                            start=(kc == 0), stop=(kc == 7)),
                            r=[("slab", s % 2), "cT"], w=["ps_mod"], signal=(kc == 7))
            for j in range(3):
                S.op("dve", lambda e, j=j: e.tensor_tensor(out=modT[:, :, j], in0=ps_mod[:, :, j], in1=bmT[:],
                                                            op=ALU.add), r=["bmT"], w=["modT", "ps_mod"], small=True)
            for m, gT in ((0, g1T), (1, g2T)):
                sc = modT[:, 8 + 24 * m: 16 + 24 * m, :]
                S.op("dve", lambda e, m=m, sc=sc, gT=gT: e.scalar_tensor_tensor(
                    out=gsT[:, m], in0=sc, scalar=1.0, in1=gT[:, :].unsqueeze(2).to_broadcast([128, 8, 3]),
                    op0=ALU.add, op1=ALU.mult), r=["modT", "g1T", "g2T"], w=["gsT"], small=True)
            S.barrier()
        if dbg and "modT" in dbg:
            S.dma("sp", dbg_d["modT"][:, :, :], modT[:], r=["modT"], chan="dbg")

        for b in range(nb if KPRE >= 3 else 0):
            build_batch(nc, S, es, sb, ps, b, locals(), dbg, dbg_d, stages)

        S.wait_all("sp")
    nc._declared_inputs = declared
    return nc


def build_batch(nc, S, es, sb, ps, b, G, dbg, dbg_d, stages):
    ident_f, ident_b, tri_f, ones_b, ones_f = (G[k] for k in ("ident_f", "ident_b", "tri_f", "ones_b", "ones_f"))
    modT, gsT, epsc = G["modT"], G["gsT"], G["epsc"]
    x_d, ctx_d, y_d, pscr_d = G["x_d"], G["ctx_d"], G["y_d"], G["pscr_d"]

    def tile_src(i):
        return ctx_d[b, i * 128:(i + 1) * 128, :] if i < 2 else x_d[b, (i - 2) * 128:(i - 1) * 128, :]

    def rms_rstd(eng_sq, xt_ap, rstd, junk, key_x, key_r):
        S.op("act", lambda e: e.activation(out=junk, in_=xt_ap, func=AF.Square, accum_out=rstd),
             r=[key_x], w=[key_r, "junk"], small=True)
        S.op("act", lambda e: e.activation(out=rstd, in_=rstd, func=AF.Sqrt, bias=epsc[:, 0:1], scale=1.0 / D),
             r=[key_r, "epsc"], w=[key_r], small=True)
        S.op("dve", lambda e: e.reciprocal(out=rstd, in_=rstd), r=[key_r], w=[key_r], small=True)

    with contextlib.ExitStack() as sbat:
        mixT = sb("mixT", [128, 8, SEQ], BF16, stack=sbat)
        hx2T = mixT
        comb = sb("comb", [128, 16, 32], stack=sbat)
        big32 = sb("big32", [128, 32], stack=sbat)
        G = dict(G)
        G["big32"] = big32
        with contextlib.ExitStack() as sm:
            if "P" in stages:
                stage_P_NA(nc, S, sm, sb, ps, b, G, dbg, dbg_d, stages, tile_src, rms_rstd, mixT)
            if "RW" in stages:
                stage_RW(nc, S, sb, ps, b, G, dbg, dbg_d, mixT)
            if "O" in stages:
                stage_O(nc, S, sb, ps, b, G, dbg, dbg_d, mixT, hx2T, comb, rms_rstd)
            S.barrier()
        if "MOE" in stages:
            stage_MOE(nc, S, sb, ps, b, G, dbg, dbg_d, hx2T, comb)
        S.barrier()


def stage_P_NA(nc, S, sm, sb, ps, b, G, dbg, dbg_d, stages, tile_src, rms_rstd, mixT):
    ident_b, ident_f, ones_b = G["ident_b"], G["ident_f"], G["ones_b"]
    modT, gsT = G["modT"], G["gsT"]
    pscr_d = G["pscr_d"]
    with contextlib.ExitStack() as st:
        qT = sb("qT", [128, 4, SEQ], BF16, stack=st)
        kT = sb("kT", [128, 4, CTX + SEQ], BF16, stack=st)
        vE = sb("vE", [128, NT, 8, 65], BF16, stack=st)
        stage_P(nc, S, sb, ps, b, G, dbg, dbg_d, tile_src, rms_rstd, qT, kT, vE)
        if "NA" in stages:
            stage_NA(nc, S, sb, ps, b, G, dbg, dbg_d, qT, kT, vE, mixT)
        S.barrier()


def stage_P(nc, S, sb, ps, b, G, dbg, dbg_d, tile_src, rms_rstd, qT, kT, vE):
    import os
    ident_b, ident_f, ones_b = G["ident_b"], G["ident_f"], G["ones_b"]
    modT, gsT = G["modT"], G["gsT"]
    pscr_d = G["pscr_d"]
    with contextlib.ExitStack() as st:
        w_in = sb("w_in", [128, 8, DIN], BF16, stack=st)
        qgr = sb("qgr", [128, 64], stack=st)
        kgr = sb("kgr", [128, 64], stack=st)
        xt = [sb("xt%d" % i, [128, D], stack=st) for i in range(2)]
        xn = sb("xn", [128, D], BF16, stack=st)
        junk = sb("junk", [128, D], BF16, stack=st)
        hxT = sb("hxT", [128, 8, 128], BF16, stack=st)
        rstd = sb("rstd", [128, 2], stack=st)
        prw = [sb("prw%d" % i, [128, DRW], stack=st) for i in range(2)]
        qk32 = sb("qk32", [128, 2, 512], stack=st)
        qkn = sb("qkn", [128, 2, 512], BF16, stack=st)
        ssq = sb("ssq", [128, 16], stack=st)
        zrow = sb("zrow", [4, DRW], stack=st)
        eps64 = sb("eps64", [128, 1], stack=st)
        ps_t = ps("ps_t", [128, 8, 128], BF16, stack=st)
        ps_p = [ps("ps_p%d" % i, [128, 512], stack=st) for i in range(4)]
        ps_qt = ps("ps_qt", [128, 8, 128], BF16, stack=st)

        wv = G["win_d"].rearrange("(kc p) n -> p kc n", p=128)
        for kc in range(8):
            S.dma("pool", w_in[:, kc, :], wv[:, kc, :], w=[("w_in", kc)], chan=("w_in", kc % 4))
        S.dma("sp", qgr[:], G["qg_d"].partition_broadcast(128), w=["qgr"])
        S.dma("sp", kgr[:], G["kg_d"].partition_broadcast(128), w=["kgr"])
        S.op("dve", lambda e: e.tensor_scalar(out=qgr[:], in0=qgr[:], scalar1=0.125, scalar2=None, op0=ALU.mult),
             r=["qgr"], w=["qgr"], small=True)
        S.op("dve", lambda e: e.memset(eps64[:], 1e-6), w=["eps64"], small=True)
        S.op("pool", lambda e: e.memset(vE[:, :, :, 64:65], 1.0), w=["vE"])
        S.op("pool", lambda e: e.memset(zrow[:], 0.0), w=["zrow"])
        for r0 in (0, 257, 258, 2307):
            S.dma("sp", pscr_d[b, r0:r0 + 1, :], zrow[0:1, :], r=["zrow"], chan="zr")

        import os
        for i in range(int(os.environ.get('KNT', NT))):
            xb_ = xt[i % 2]
            kx = ("xt", i % 2)
            S.dma("sp", xb_[:], tile_src(i), w=[kx])
            rs = rstd[:, 0:1]
            rms_rstd("act", xb_[:], rs, junk[:], kx, "rstd")
            KSUB = int(os.environ.get('KSUB', 9))
            if KSUB < 2:
                continue
            S.op("dve", lambda e: e.tensor_scalar(out=xn[:], in0=xb_[:], scalar1=rs, scalar2=None, op0=ALU.mult),
                 r=[kx, "rstd"], w=["xn"])
            for j in range(8):
                S.op("pe", lambda e, j=j: e.transpose(out=ps_t[:, j, :], in_=xn[:, j * 128:(j + 1) * 128],
                                                      identity=ident_b[:]), r=["xn", "ident_b"], w=["ps_t"],
                     signal=(j == 7))
            mb = 2 if i < 2 else b
            KEV = os.environ.get('KEV', 'AD')
            for j in range(8):
                eng = "act" if i % 2 == 0 else "dve"
                if eng == "act":
                    S.op("act", lambda e, j=j: e.activation(out=hxT[:, j, :], in_=ps_t[:, j, :], func=AF.Identity,
                                                            bias=modT[:, j, mb:mb + 1], scale=gsT[:, 0, j, mb:mb + 1]),
                         r=["modT", "gsT"], w=[("hxT", j), "ps_t"])
                else:
                    S.op("dve", lambda e, j=j: e.tensor_scalar(out=hxT[:, j, :], in0=ps_t[:, j, :],
                                                               scalar1=gsT[:, 0, j, mb:mb + 1],
                                                               scalar2=modT[:, j, mb:mb + 1], op0=ALU.mult, op1=ALU.add),
                         r=["modT", "gsT"], w=[("hxT", j), "ps_t"])
            if KSUB < 3:
                continue
            pr = prw[i % 2]
            kpr = ("prw", i % 2)
            for n in range(7):
                ncol = 512 if n < 6 else DIN - 6 * 512
                pp = ps_p[n % 4]
                kp = ("ps_p", n % 4)
                for kc in range(8):
                    S.op("pe", lambda e, n=n, kc=kc, pp=pp, ncol=ncol: e.matmul(
                        pp[:, 0:ncol], lhsT=hxT[:, kc, :], rhs=w_in[:, kc, n * 512:n * 512 + ncol],
                        start=(kc == 0), stop=(kc == 7)), r=[("hxT", kc), ("w_in", kc)], w=[kp], signal=(kc == 7))
                if n < 2:
                    if (n == 0 and i < 2) or KSUB < 4:
                        continue
                    gr = qgr if n == 0 else kgr
                    S.op("act", lambda e, n=n, pp=pp: e.activation(out=qk32[:, n, :], in_=pp[:], func=AF.Copy),
                         w=[("qk32", n), kp])
                    S.op("dve", lambda e, n=n, pp=pp: e.tensor_tensor(out=junk[:, 0:512], in0=pp[:], in1=qk32[:, n, :],
                                                                      op=ALU.mult), r=[("qk32", n)], w=["junk", kp])
                    sq = ssq[:, n * 8:(n + 1) * 8]
                    S.op("dve", lambda e, sq=sq: e.tensor_reduce(out=sq, in_=junk[:, 0:512].rearrange("p (h d) -> p h d", d=64),
                                                                 axis=AX.X, op=ALU.add), r=["junk"], w=["ssq"], small=True)
                    S.op("act", lambda e, sq=sq: e.activation(out=sq, in_=sq, func=AF.Sqrt, bias=eps64[:, 0:1], scale=1.0 / 64),
                         r=["ssq", "eps64"], w=["ssq"], small=True)
                    S.op("dve", lambda e, sq=sq: e.reciprocal(out=sq, in_=sq), r=["ssq"], w=["ssq"], small=True)
                    q3 = qk32[:, n, :].rearrange("p (h d) -> p h d", d=64)
                    S.op("dve", lambda e, sq=sq, q3=q3: e.tensor_tensor(out=q3, in0=q3, in1=sq.unsqueeze(2).to_broadcast([128, 8, 64]),
                                                                        op=ALU.mult), r=["ssq", ("qk32", n)], w=[("qk32", n)])
                    S.op("pool", lambda e, n=n, q3=q3, gr=gr: e.tensor_tensor(
                        out=qkn[:, n, :].rearrange("p (h d) -> p h d", d=64), in0=q3,
                        in1=gr[:, :].unsqueeze(1).to_broadcast([128, 8, 64]), op=ALU.mult),
                        r=[("qk32", n), "qgr", "kgr"], w=[("qkn", n)])
                    for pr_ in range(4):
                        S.op("pe", lambda e, n=n, pr_=pr_: e.transpose(out=ps_qt[:, n * 4 + pr_, :],
                                                                       in_=qkn[:, n, pr_ * 128:(pr_ + 1) * 128],
                                                                       identity=ident_b[:]),
                             r=[("qkn", n), "ident_b"], w=["ps_qt"], signal=(pr_ == 3))
                    if n == 0:
                        S.op("act", lambda e, i=i: e.activation(out=qT[:, :, (i - 2) * 128:(i - 1) * 128], in_=ps_qt[:, 0:4, :],
                                                                func=AF.Copy), w=["qT", "ps_qt"])
                    else:
                        S.op("act", lambda e, i=i: e.activation(out=kT[:, :, i * 128:(i + 1) * 128], in_=ps_qt[:, 4:8, :],
                                                                func=AF.Copy), w=["kT", "ps_qt"])
                elif n == 2:
                    S.op("act", lambda e, i=i, pp=pp: e.activation(out=vE[:, i, :, 0:64],
                                                                   in_=pp[:].rearrange("p (h d) -> p h d", d=64),
                                                                   func=AF.Copy), w=["vE", kp])
                else:
                    c0 = (n - 3) * 512
                    eng = "dve" if n % 2 == 1 else "act"
                    if eng == "dve":
                        S.op("dve", lambda e, pp=pp, c0=c0, ncol=ncol, pr=pr: e.tensor_copy(out=pr[:, c0:c0 + ncol], in_=pp[:, 0:ncol]),
                             w=[kpr, kp])
                    else:
                        S.op("act", lambda e, pp=pp, c0=c0, ncol=ncol, pr=pr: e.activation(out=pr[:, c0:c0 + ncol], in_=pp[:, 0:ncol],
                                                                                           func=AF.Copy), w=[kpr, kp])
            t0 = CTX_BASE + 128 * i if i < 2 else LAT_BASE + 128 * (i - 2)
            S.dma("sp", pscr_d[b, t0:t0 + 128, :], pr[:], r=[kpr], w=[("pscr", i)], chan=("pscw", i % 2))
        if dbg and "kT" in dbg and b == 0:
            S.dma("sp", dbg_d["kT"][:, :, :], kT[:], r=["kT"], chan="dbg")
            S.dma("sp", dbg_d["qT"][:, :, :], qT[:], r=["qT"], chan="dbg")
            S.dma("sp", dbg_d["vE"][:, :, :, :], vE[:], r=["vE"], chan="dbg")
        S.barrier()


def stage_NA(nc, S, sb, ps, b, G, dbg, dbg_d, qT, kT, vE, mixT):
    ident_b, ones_b = G["ident_b"], G["ones_b"]
    with contextlib.ExitStack() as st:
        mtab = sb("mtab", [128, 32, 480], BF16, stack=st)
        rmask = sb("rmask", [66, 32 * 128], BF16, stack=st)
        sel2 = sb("sel2", [66, 128], BF16, stack=st)
        natok = sb("natok", [128, 512], BF16, stack=st)
        pT = [sb("pT%d" % i, [128, 10, 128], BF16, stack=st) for i in range(2)]
        rden = sb("rden", [128, 2], stack=st)
        psw = [ps("psw%d" % i, [128, 1536], stack=st) for i in range(2)]
        psO = ps("psO", [128, 128], stack=st)
        psT = ps("psT", [128, 4, 128], BF16, stack=st)
        S.dma("pool", mtab[:], G["mtab_d"][:, :, :], w=["mtab"])
        for pb_ in (0, 64):
            S.dma("pool", rmask[pb_:pb_ + 2, :], G["rmask_d"][:, :], w=[("rmask", pb_)], chan=("rmaskA", pb_))
            S.dma("pool", sel2[pb_:pb_ + 2, :], G["sel2_d"][:, :], w=[("sel2", pb_)], chan=("rmaskB", pb_))
        r0s = [0, 4, 12, 16]
        import os
        KNA = int(os.environ.get("KNA", 9))
        KNAB = int(os.environ.get("KNAB", 16))
        def emit_scores(rb, cb, h):
            pair, hb = h // 2, (h % 2) * 64
            pw = psw[h % 2]
            kw_ = ("psw", h % 2)
            pt = pT[h % 2]
            kpt = ("pT", h % 2)
            qblk = qT[hb:hb + 64, pair, :].rearrange("p (r c) -> p r c", c=64)[:, 8 * rb:8 * rb + 8, 16 * cb:16 * cb + 16]
            for c in range(2):
                S.op("pe", lambda e, c=c: e.matmul(pw[:, c * 128:(c + 1) * 128], lhsT=kT[hb:hb + 64, pair, c * 128:(c + 1) * 128],
                                                   rhs=qblk, start=True, stop=True), r=["kT", "qT"], w=[kw_], signal=False)
            for sl_ in range(8):
                kr_e = r0s[rb] + 2 * sl_
                ktok = CTX + kr_e * 64
                e0 = 14 - (kr_e - 8 * rb)
                out = pw[:, (2 + sl_) * 128:(3 + sl_) * 128]
                S.op("pe", lambda e, out=out, ktok=ktok: e.matmul(out, lhsT=kT[hb:hb + 64, pair, ktok:ktok + 128], rhs=qblk, start=True, stop=False),
                     r=["kT", "qT"], w=[kw_], signal=False)
                S.op("pe", lambda e, out=out, e0=e0: e.matmul(out, lhsT=ident_b[:, :], rhs=mtab[:, h * 4 + cb, e0 * 16:e0 * 16 + 128], start=False, stop=False),
                     r=["mtab", "ident_b"], w=[kw_], signal=False)
                S.op("pe", lambda e, out=out, sl_=sl_: e.matmul(out, lhsT=sel2[hb:hb + 2, :], rhs=rmask[hb:hb + 2, (rb * 8 + sl_) * 128:(rb * 8 + sl_ + 1) * 128],
                                                                start=False, stop=True), r=[("rmask", hb), ("sel2", hb)], w=[kw_], signal=(sl_ == 7))
            for bk in range(3):
                c0_, c1_ = bk * 512, min(bk * 512 + 512, 1280)
                S.op("act", lambda e, c0_=c0_, c1_=c1_: e.activation(out=pt[:].rearrange("p a b -> p (a b)")[:, c0_:c1_], in_=pw[:, c0_:c1_], func=AF.Exp),
                     w=[kpt, kw_])

        def emit_pv(rb, cb, h):
            pt = pT[h % 2]
            kpt = ("pT", h % 2)
            for sl_ in range(8):
                tile_ = 2 + (r0s[rb] + 2 * sl_) // 2
                S.op("pe", lambda e, sl_=sl_, tile_=tile_: e.matmul(psO[:, 0:65], lhsT=pt[:, 2 + sl_, :], rhs=vE[:, tile_, h, :],
                                                                    start=(sl_ == 0), stop=False), r=[kpt, "vE"], w=["psO"], signal=False)
            for c in range(2):
                S.op("pe", lambda e, c=c: e.matmul(psO[:, 0:65], lhsT=pt[:, c, :], rhs=vE[:, c, h, :],
                                                   start=False, stop=(c == 1)), r=[kpt, "vE"], w=["psO"], signal=(c == 1))
            S.op("dve", lambda e: e.reciprocal(out=rden[:, 0:1], in_=psO[:, 64:65]), w=["rden", "psO"], small=True)
            S.op("dve", lambda e: e.tensor_scalar(out=natok[:, h * 64:(h + 1) * 64], in0=psO[:, 0:64], scalar1=rden[:, 0:1],
                                                  scalar2=None, op0=ALU.mult), r=["rden"], w=["natok", "psO"], small=True)
            if h == 7:
                for pr_ in range(4):
                    S.op("pe", lambda e, pr_=pr_: e.transpose(out=psT[:, pr_, :], in_=natok[:, pr_ * 128:(pr_ + 1) * 128],
                                                              identity=ident_b[:]), r=["natok", "ident_b"], w=["psT"], signal=(pr_ == 3))
                mo = mixT[:, 0:4, :].rearrange("p a (r c) -> p a r c", c=64)[:, :, 8 * rb:8 * rb + 8, 16 * cb:16 * cb + 16]
                S.op("act", lambda e, mo=mo: e.activation(out=mo, in_=psT[:].rearrange("p a (r c) -> p a r c", c=16), func=AF.Copy),
                     w=["mixT_na", "psT"])

        items = [(rb, cb, h) for rb in range(4) for cb in range(4) for h in range(8) if rb * 4 + cb < KNAB]
        emit_scores(*items[0])
        for k_ in range(len(items)):
            if k_ + 1 < len(items):
                emit_scores(*items[k_ + 1])
            emit_pv(*items[k_])
        if dbg and "naT" in dbg and b == 0 and KNA >= 7 and KNAB >= 16:
            S.dma("sp", dbg_d["naT"][:, :, :], mixT[:, 0:4, :], r=["mixT_na"], chan="dbg")
        S.barrier()


def _host_prep(inp):
    f32 = np.float32
    L = 0
    c = inp["c"].astype(f32)
    c_ctx = inp["c_ctx"].astype(f32)
    shared = {}
    shared["w_mod"] = np.ascontiguousarray(inp["w_mod"][L])
    shared["bmodT"] = np.ascontiguousarray(inp["b_mod"][L].reshape(48, 128).T)
    shared["g1T"] = np.ascontiguousarray(inp["norm1_g"][L].reshape(8, 128).T)
    shared["g2T"] = np.ascontiguousarray(inp["norm2_g"][L].reshape(8, 128).T)
    shared["w_in"] = np.ascontiguousarray(inp["w_in"][L])
    shared["na_q_g"] = np.ascontiguousarray(inp["na_q_g"][L])
    shared["na_k_g"] = np.ascontiguousarray(inp["na_k_g"][L])
    rpb = inp["na_rpb"][L].astype(f32)
    j = np.arange(64)[:, None, None, None]
    cb = np.arange(4)[None, :, None, None]
    e = np.arange(30)[None, None, :, None]
    qc = np.arange(16)[None, None, None, :]
    ri = 21 - e + 0 * j + 0 * cb + 0 * qc
    qcol = 16 * cb + qc
    ci = j - qcol + 15 + 0 * e
    cs = np.clip(qcol - 8, 0, 48)
    colok = (j >= cs) & (j < cs + 16)
    ok = (ri >= 0) & (ri < 15) & (ci >= 0) & (ci < 31) & (colok | (e < 0))
    ric = np.clip(ri, 0, 14)
    cic = np.clip(ci, 0, 30)
    mt = np.empty((64, 8, 4, 30, 16), f32)
    for h in range(8):
        mt[:, h] = np.where(ok, rpb[h][ric, cic], f32(NEGM))
    mtsh = np.full_like(mt, f32(NEGM))
    mtsh[:, :, :, 1:, :] = mt[:, :, :, :-1, :]
    shared["mtab"] = np.ascontiguousarray(np.concatenate([mt.reshape(64, 32, 480), mtsh.reshape(64, 32, 480)], axis=0))
    rm = np.full((4, 15, 8), NEGM, f32)
    r0s = [0, 4, 12, 17]
    for rb in range(4):
        for jr in range(15):
            kr = r0s[rb] + jr
            for qr in range(8):
                qrow = rb * 8 + qr
                rs_ = min(max(qrow - 4, 0), 24)
                if rs_ <= kr < rs_ + 8:
                    rm[rb, jr, qr] = 0.0
    rm2 = np.full((2, 4, 8, 8), NEGM, f32)
    r0e = [0, 4, 12, 16]
    for rb in range(4):
        for sl_ in range(8):
            for par in range(2):
                kr = r0e[rb] + 2 * sl_ + par
                for qr in range(8):
                    qrow = rb * 8 + qr
                    rs_ = min(max(qrow - 4, 0), 24)
                    if rs_ <= kr < rs_ + 8:
                        rm2[par, rb, sl_, qr] = 0.0
    shared["rmask"] = np.ascontiguousarray(np.repeat(rm2[:, :, :, :, None], 16, axis=4).reshape(2, 32 * 128))
    sel2 = np.zeros((2, 128), f32)
    sel2[0, 0:64] = 1.0
    sel2[1, 64:128] = 1.0
    shared["sel2"] = sel2
    nf = 16
    pos = np.arange(SEQ)
    inv = (10000.0 ** (-np.arange(nf, dtype=np.float32) / nf)).astype(f32)
    ang = np.stack([(pos // 64).astype(f32)[:, None] * inv, (pos % 64).astype(f32)[:, None] * inv], axis=1)
    cs_ = np.concatenate([np.cos(ang).astype(f32), np.sin(ang).astype(f32)], axis=-1)
    shared["rope"] = np.ascontiguousarray(cs_.reshape(16, 128, 2, 32).transpose(1, 0, 2, 3))
    ii = np.arange(128)
    U = (ii[:, None] < ii[None, :]).astype(f32)
    Ui = (ii[:, None] <= ii[None, :]).astype(f32)
    shared["tri"] = np.ascontiguousarray(np.stack([U, Ui, U.T, Ui.T], axis=1))
    shared["ident"] = np.eye(128, dtype=f32)
    blk = [(ii[:, None] // 16 == ii[None, :] // 16)]
    for l_ in range(3):
        blk.append((ii[:, None] // (32 << l_) == ii[None, :] // (32 << l_)) & (ii[:, None] // (16 << l_) != ii[None, :] // (16 << l_)))
    shared["blk"] = np.ascontiguousarray(np.stack(blk, axis=1).astype(f32))
    for k in ("rw_mu_prev", "rw_mu_next", "rw_w_up", "rw_a_up", "rw_g_up", "rw_k_k", "rw_k_a", "rw_ln_g", "rw_ln_b", "w_out"):
        shared[k] = np.ascontiguousarray(inp[k][L])
    shared["rw_w0"] = np.ascontiguousarray(inp["rw_w0"][L].reshape(-1))
    shared["rw_a0"] = np.ascontiguousarray(inp["rw_a0"][L].reshape(-1))
    shared["rw_r_k"] = np.ascontiguousarray(inp["rw_r_k"][L].reshape(-1))
    we = inp["moe_we"][L]
    shared["wcat"] = np.ascontiguousarray(np.concatenate([inp["moe_wg"][L], we.transpose(1, 0, 2).reshape(D, 32)], axis=1))
    shared["bcat"] = np.ascontiguousarray(np.concatenate([inp["moe_bg"][L], inp["moe_be"][L].reshape(-1)]))
    shared["moe_w1"] = np.ascontiguousarray(inp["moe_w1"][L])
    shared["moe_w3"] = np.ascontiguousarray(inp["moe_w3"][L])
    shared["moe_w2"] = np.ascontiguousarray(inp["moe_w2"][L])
    in_maps = []
    for core in range(8):
        m = dict(shared)
        bs = slice(core * NB, (core + 1) * NB)
        m["x"] = np.ascontiguousarray(inp["x"][bs])
        m["ctx"] = np.ascontiguousarray(inp["ctx"][bs])
        cc = np.stack([c[core * NB], c[core * NB + 1], c_ctx], axis=-1)
        m["cT"] = np.ascontiguousarray(cc.reshape(8, 128, 3).transpose(1, 0, 2))
        in_maps.append(m)
    return in_maps


def kernel(**inputs):
    inp = {k: np.asarray(v) for k, v in inputs.items()}
    in_maps = _host_prep(inp)
    nc = build()
    res = run_bass_kernel_spmd(nc, in_maps, core_ids=list(range(8)))
    out = np.concatenate([r["y"] for r in res.results], axis=0)
    return out.astype(np.float32)


def stage_RW(nc, S, sb, ps, b, G, dbg, dbg_d, mixT):
    import os
    ident_b, ident_f, ones_b, ones_f, tri_f = G["ident_b"], G["ident_f"], G["ones_b"], G["ones_f"], G["tri_f"]
    pscr_d = G["pscr_d"]
    C = CDEC
    with contextlib.ExitStack() as st:
        def T(name, shape, dt=F32):
            return sb(name, shape, dt, stack=st)
        mup, mun = T("mup", [128, DRW]), T("mun", [128, DRW])
        w0r, a0r = T("w0r", [128, 1, 512]), T("a0r", [128, 1, 512])
        kkr, kar, omkar, rkr, lngr, lnbr = (T(n, [128, 512]) for n in ("kkr", "kar", "omkar", "rkr", "lngr", "lnbr"))
        wup, aup, gup = T("wup", [64, 2, 512], BF16), T("aup", [64, 2, 512], BF16), T("gup", [128, 512], BF16)
        rope = T("rope", [128, 16, 2, 32])
        ntri = T("ntri", [128, 4, 128])
        idp = T("idp", [128, 64])
        blkb = T("blkb", [128, 4, 128], BF16)
        oacc = T("oacc", [128, 16, 512])
        bon = T("bon", [128, 16, 512], BF16)
        sgd = T("sgd", [128, 16, 128], BF16)
        Hs = [T("H%d" % d, [128, 4, 64], BF16) for d in range(2)]
        p0, pm, pp = T("p0", [128, DRW]), T("pm", [128, DRW]), T("pp", [128, DRW])
        rk2 = T("rk2", [128, 1024])
        tA, tB = T("tA", [128, 512]), T("tB", [128, 512])
        vb = T("vb", [128, 512], BF16)
        lin = T("lin", [128, 256], BF16)
        lT = T("lT", [128, 3, 128], BF16)
        aa = T("aa", [128, 512])
        lw = pp[:, 1024:1536]
        kkn, akk, kw = T("kkn", [128, 512]), T("akk", [128, 512]), T("kw", [128, 512])
        st8 = T("st8", [128, 32])
        Eneg, Epos, Eex = pm[:, 0:512], pm[:, 512:1024], pm[:, 1024:1536]
        Ehat, cum = pp[:, 0:512], pp[:, 512:1024]
        PCf = T("PCf", [128, 4])
        tok6 = T("tok6", [128, 6, 512], BF16)
        fT = T("fT", [128, 4, 4, 128], BF16)
        prod = T("prod", [128, 5, 8, 128], BF16)
        XY2 = T("XY2", [128, 2, 8, 128], BF16)
        Tt = T("Tt", [128, 8, 128], BF16)
        Zs, nW, Abs_ = T("Zs", [128, 512], BF16), T("nW", [128, 512], BF16), T("Abs", [128, 512], BF16)
        RbT = T("RbT", [128, 4, 128], BF16)
        Gt = T("Gt", [128, 4, 64], BF16)
        rwtok = T("rwtok", [128, 512], BF16)
        pb = [ps("pb%d" % i, [128, 512], stack=st) for i in range(6)]
        pt = [ps("pt%d" % i, [128, 8, 128], BF16, stack=st) for i in range(2)]
        KB = [("pb", i) for i in range(6)]
        KT = [("pt", i) for i in range(2)]

        def op(eng, fn, r=(), w=(), small=False, signal=True):
            return S.op(eng, fn, r=r, w=w, small=small, signal=signal)

        for dst, src in ((mup, G["mup_d"]), (mun, G["mun_d"]), (kkr, G["kk_d"]), (kar, G["ka_d"]), (rkr, G["rk_d"]),
                         (lngr, G["lng_d"]), (lnbr, G["lnb_d"])):
            S.dma("sp", dst[:], src.partition_broadcast(128), w=["rwc"], chan="rwc")
        S.dma("sp", rope[:], G["rope_d"][:, :, :, :], w=["rwc"], chan="rwc")
        S.dma("pool", wup[:], G["wup_d"].rearrange("d k n -> k d n"), w=["rwc2"], chan="rwc2")
        S.dma("pool", aup[:], G["aup_d"].rearrange("d k n -> k d n"), w=["rwc2"], chan="rwc2")
        S.dma("pool", gup[:], G["gup_d"][:, :], w=["rwc2"], chan="rwc2")
        S.dma("pool", blkb[:], G["blk_d"][:, :, :], w=["blkb"], chan="rwc2")
        op("dve", lambda e: e.tensor_scalar(out=omkar[:], in0=kar[:], scalar1=-1.0, scalar2=1.0, op0=ALU.mult, op1=ALU.add),
           r=["rwc"], w=["omkar"])
        op("dve", lambda e: e.tensor_scalar(out=ntri[:], in0=tri_f[:], scalar1=-1.0, scalar2=None, op0=ALU.mult), r=["tri_f"], w=["ntri"])
        op("dve", lambda e: e.tensor_copy(out=idp[0:64, :], in_=ident_f[0:64, 0:64]), r=["ident_f"], w=["idp"])
        op("dve", lambda e: e.tensor_copy(out=idp[64:128, :], in_=ident_f[64:128, 64:128]), r=["ident_f"], w=["idp"])
        op("pool", lambda e: e.memset(oacc[:], 0.0), w=["oacc"])

        nvis = int(os.environ.get("KRW", 99))
        for d in range(int(os.environ.get("KDIR", 2))):
            H = Hs[d]
            kH = ("H", d)
            op("pool", lambda e, H=H: e.memset(H[:], 0.0), w=[kH])
            S.dma("sp", w0r[:, 0, :], G["w0_d"][d * 512:(d + 1) * 512].partition_broadcast(128), w=["w0a0"], chan="rwc")
            S.dma("sp", a0r[:, 0, :], G["a0_d"][d * 512:(d + 1) * 512].partition_broadcast(128), w=["w0a0"], chan="rwc")
            order = list(range(NT)) if d == 0 else [1, 0] + list(range(NT - 1, 1, -1))
            mX, mY, mI = (0, 2, 1) if d == 0 else (2, 0, 3)
            for vi, i in enumerate(order[:nvis]):
                lat = i >= 2
                j = i - 2
                t0 = CTX_BASE + 128 * i if i < 2 else LAT_BASE + 128 * j
                S.dma("sp", p0[:], pscr_d[b, t0:t0 + 128, :], r=[("pscr", i)], w=["p0"])
                S.dma("sp", pm[:], pscr_d[b, t0 - 1:t0 + 127, :], r=[("pscr", i), ("pscr", max(i - 1, 0))], w=["pm", "Eneg", "Epos", "Eex"])
                S.dma("sp", pp[:], pscr_d[b, t0 + 1:t0 + 129, :], r=[("pscr", i), ("pscr", min(i + 1, NT - 1))], w=["pp", "Ehat", "cum", "lw"])
                op("dve", lambda e: e.tensor_tensor(out=pm[:], in0=pm[:], in1=p0[:], op=ALU.subtract), r=["p0"], w=["pm", "Eneg", "Epos", "Eex"])
                op("pool", lambda e: e.tensor_tensor(out=pp[:], in0=pp[:], in1=p0[:], op=ALU.subtract), r=["p0"], w=["pp", "Ehat", "cum", "lw"])
                op("dve", lambda e: e.tensor_tensor(out=pm[:], in0=pm[:], in1=mup[:], op=ALU.mult), r=["rwc"], w=["pm", "Eneg", "Epos", "Eex"])
                op("pool", lambda e: e.tensor_tensor(out=pp[:], in0=pp[:], in1=mun[:], op=ALU.mult), r=["rwc"], w=["pp", "Ehat", "cum", "lw"])
                op("dve", lambda e: e.tensor_tensor(out=p0[:], in0=p0[:], in1=pm[:], op=ALU.add), r=["pm"], w=["p0"])
                op("dve", lambda e: e.tensor_tensor(out=p0[:], in0=p0[:], in1=pp[:], op=ALU.add), r=["pp"], w=["p0"])
                if lat:
                    s5 = p0[:, 0:1024].rearrange("p (h a t f) -> p h a t f", a=2, t=2, f=16)
                    d5 = rk2[:].rearrange("p (h a t f) -> p h a t f", a=2, t=2, f=16)
                    cs = rope[:, j, :, 0:16].unsqueeze(1).to_broadcast([128, 16, 2, 16])
                    sn = rope[:, j, :, 16:32].unsqueeze(1).to_broadcast([128, 16, 2, 16])
                    a4 = tA[:, 0:512].rearrange("p (h a f) -> p h a f", a=2, f=16)
                    b4 = tB[:, 0:512].rearrange("p (h a f) -> p h a f", a=2, f=16)
                    t1, t2 = s5[:, :, :, 0, :], s5[:, :, :, 1, :]
                    op("dve", lambda e: e.tensor_tensor(out=d5[:, :, :, 0, :], in0=t1, in1=cs, op=ALU.mult), r=["p0", "rwc"], w=["rk2a"])
                    op("pool", lambda e: e.tensor_tensor(out=a4, in0=t2, in1=sn, op=ALU.mult), r=["p0", "rwc"], w=["tA"])
                    op("dve", lambda e: e.tensor_tensor(out=d5[:, :, :, 0, :], in0=d5[:, :, :, 0, :], in1=a4, op=ALU.subtract), r=["tA"], w=["rk2a"])
                    op("pool", lambda e: e.tensor_tensor(out=d5[:, :, :, 1, :], in0=t2, in1=cs, op=ALU.mult), r=["p0", "rwc"], w=["rk2b"])
                    op("dve", lambda e: e.tensor_tensor(out=b4, in0=t1, in1=sn, op=ALU.mult), r=["p0", "rwc"], w=["tB"])
                    op("pool", lambda e: e.tensor_tensor(out=d5[:, :, :, 1, :], in0=d5[:, :, :, 1, :], in1=b4, op=ALU.add), r=["tB"], w=["rk2b"])
                    rr, kr_ = rk2[:, 0:512], rk2[:, 512:1024]
                    krk = ["rk2a", "rk2b"]
                else:
                    rr, kr_ = p0[:, 0:512], p0[:, 512:1024]
                    krk = ["p0"]
                v_ = p0[:, 1024:1536]
                KRWL = int(os.environ.get("KRWL", 9))
                if KRWL < 2:
                    continue
                op("act", lambda e: e.activation(out=vb[:], in_=v_, func=AF.Copy), r=["p0"], w=["vb"])
                op("act", lambda e: e.activation(out=lin[:, 0:64], in_=p0[:, 1536 + 64 * d:1600 + 64 * d], func=AF.Tanh), r=["p0"], w=["lin"])
                op("act", lambda e: e.activation(out=lin[:, 64:128], in_=p0[:, 1664 + 64 * d:1728 + 64 * d], func=AF.Copy), r=["p0"], w=["lin"])
                dog = lat and d == 0
                if dog:
                    op("act", lambda e: e.activation(out=sgd[:, j, :], in_=p0[:, 1792:1920], func=AF.Sigmoid), r=["p0"], w=["sgd"])
                op("pe", lambda e: e.transpose(out=pt[0][0:64, 0, :], in_=lin[:, 0:64], identity=ident_b[:]), r=["lin"], w=[KT[0]], signal=False)
                op("pe", lambda e: e.transpose(out=pt[0][0:64, 1, :], in_=lin[:, 64:128], identity=ident_b[:]), r=["lin"], w=[KT[0]])
                op("act", lambda e: e.activation(out=lT[0:64, 0:2, :], in_=pt[0][0:64, 0:2, :], func=AF.Copy), w=["lT", KT[0]])
                op("pe", lambda e: e.matmul(pb[0][:], lhsT=lT[0:64, 0, :], rhs=wup[0:64, d, :], start=True, stop=True), r=["lT", "rwc2"], w=[KB[0]])
                op("pe", lambda e: e.matmul(pb[1][:], lhsT=lT[0:64, 1, :], rhs=aup[0:64, d, :], start=True, stop=True), r=["lT", "rwc2"], w=[KB[1]])
                op("dve", lambda e: e.tensor_tensor(out=lw[:], in0=pb[0][:], in1=w0r[:, 0, :], op=ALU.add), r=["w0a0"], w=["lw", KB[0]])
                op("act", lambda e: e.activation(out=lw[:], in_=lw[:], func=AF.Sigmoid), w=["lw"])
                op("dve", lambda e: e.tensor_tensor(out=aa[:], in0=pb[1][:], in1=a0r[:, 0, :], op=ALU.add), r=["w0a0"], w=["aa", KB[1]])
                op("act", lambda e: e.activation(out=aa[:], in_=aa[:], func=AF.Sigmoid), w=["aa"])
                if KRWL < 3:
                    continue
                op("dve", lambda e: e.tensor_tensor(out=kkn[:], in0=kr_, in1=kkr[:], op=ALU.mult), r=krk + ["rwc"], w=["kkn"])
                op("pool", lambda e: e.tensor_tensor(out=tA[:], in0=kkn[:], in1=kkn[:], op=ALU.mult), r=["kkn"], w=["tA"])
                op("dve", lambda e: e.tensor_reduce(out=st8[:, 0:8], in_=tA[:].rearrange("p (h f) -> p h f", f=64), axis=AX.X, op=ALU.add),
                   r=["tA"], w=["st8"], small=True)
                op("act", lambda e: e.activation(out=st8[:, 0:8], in_=st8[:, 0:8], func=AF.Sqrt), w=["st8"], small=True)
                op("dve", lambda e: e.tensor_scalar(out=st8[:, 0:8], in0=st8[:, 0:8], scalar1=1e-12, scalar2=None, op0=ALU.max), w=["st8"], small=True)
                op("dve", lambda e: e.reciprocal(out=st8[:, 0:8], in_=st8[:, 0:8]), w=["st8"], small=True)
                k3 = kkn[:].rearrange("p (h f) -> p h f", f=64)
                op("dve", lambda e: e.tensor_tensor(out=k3, in0=k3, in1=st8[:, 0:8].unsqueeze(2).to_broadcast([128, 8, 64]), op=ALU.mult),
                   r=["st8"], w=["kkn"], small=True)
                op("pool", lambda e: e.tensor_tensor(out=akk[:], in0=kkn[:], in1=aa[:], op=ALU.mult), r=["kkn", "aa"], w=["akk"])
                op("pool", lambda e: e.tensor_tensor(out=tB[:], in0=aa[:], in1=kar[:], op=ALU.mult), r=["aa", "rwc"], w=["tB"])
                op("pool", lambda e: e.tensor_tensor(out=tB[:], in0=tB[:], in1=omkar[:], op=ALU.add), r=["omkar"], w=["tB"])
                op("dve", lambda e: e.tensor_tensor(out=kw[:], in0=kr_, in1=tB[:], op=ALU.mult), r=krk + ["tB"], w=["kw"])
                if dog:
                    op("dve", lambda e: e.tensor_tensor(out=tA[:], in0=rr, in1=kr_, op=ALU.mult), r=krk, w=["tA"])
                    op("dve", lambda e: e.tensor_tensor(out=tA[:], in0=tA[:], in1=rkr[:], op=ALU.mult), r=["rwc"], w=["tA"])
                    op("dve", lambda e: e.tensor_reduce(out=st8[:, 8:16], in_=tA[:].rearrange("p (h f) -> p h f", f=64), axis=AX.X, op=ALU.add),
                       r=["tA"], w=["st8b"], small=True)
                    op("dve", lambda e: e.tensor_tensor(out=bon[:, j, :].rearrange("p (h f) -> p h f", f=64),
                                                        in0=v_.rearrange("p (h f) -> p h f", f=64),
                                                        in1=st8[:, 8:16].unsqueeze(2).to_broadcast([128, 8, 64]), op=ALU.mult),
                       r=["st8b", "p0"], w=["bon"], small=True)
                op("pe", lambda e: e.matmul(pb[3][:], lhsT=tri_f[:, mI, :], rhs=lw[:], start=True, stop=True), r=["lw", "tri_f"], w=[KB[3]])
                op("pe", lambda e: e.matmul(pb[4][:], lhsT=ones_f[:], rhs=lw[:], start=True, stop=True), r=["lw", "ones_f"], w=[KB[4]])
                for pr_ in range(4):
                    op("pe", lambda e, pr_=pr_: e.matmul(pb[5][:, pr_:pr_ + 1], lhsT=lw[:, pr_ * 128:(pr_ + 1) * 128], rhs=ones_f[:, 0:1],
                                                         start=True, stop=True), r=["lw", "ones_f"], w=[KB[5]], signal=(pr_ == 3))
                op("act", lambda e: e.activation(out=cum[:], in_=pb[3][:], func=AF.Copy), w=["cum", KB[3]])
                op("act", lambda e: e.activation(out=Eneg[:], in_=cum[:], func=AF.Exp, scale=-C), r=["cum"], w=["Eneg"])
                op("act", lambda e: e.activation(out=Epos[:], in_=cum[:], func=AF.Exp, scale=C), r=["cum"], w=["Epos"])
                op("pool", lambda e: e.tensor_tensor(out=Eex[:], in0=cum[:], in1=lw[:], op=ALU.subtract), r=["cum", "lw"], w=["Eex"])
                op("act", lambda e: e.activation(out=Eex[:], in_=Eex[:], func=AF.Exp, scale=-C), w=["Eex"])
                op("dve", lambda e: e.tensor_tensor(out=Ehat[:], in0=pb[4][:], in1=cum[:], op=ALU.subtract), r=["cum"], w=["Ehat", KB[4]])
                op("act", lambda e: e.activation(out=Ehat[:], in_=Ehat[:], func=AF.Exp, scale=-C), w=["Ehat"])
                op("act", lambda e: e.activation(out=PCf[:], in_=pb[5][:, 0:4], func=AF.Exp, scale=-C), w=["PCf", KB[5]], small=True)
                for idx, (eng, a_, b_) in enumerate((("dve", kkn, Eex), ("pool", rr, Eneg), ("dve", kw, Epos), ("pool", akk, Epos),
                                                      ("dve", kw, Ehat), ("pool", akk, Ehat))):
                    a_ap = a_ if not hasattr(a_, "ap") or idx == 1 else a_[:]
                    if idx == 1:
                        a_ap = rr
                    op(eng, lambda e, idx=idx, a_ap=a_ap, b_=b_: e.tensor_tensor(out=tok6[:, idx, :], in0=a_ap, in1=b_[:], op=ALU.mult),
                       r=krk + ["kkn", "kw", "akk", "Eex", "Eneg", "Epos", "Ehat"], w=[("tok6", idx)])
                if KRWL < 4:
                    continue
                for wi, src in enumerate((0, 3, 2, 1)):
                    if wi == 3 and not lat:
                        continue
                    ptt = pt[wi // 2]
                    for pr_ in range(4):
                        op("pe", lambda e, wi=wi, src=src, pr_=pr_, ptt=ptt: e.transpose(
                            out=ptt[:, (wi % 2) * 4 + pr_, :], in_=tok6[:, src, pr_ * 128:(pr_ + 1) * 128], identity=ident_b[:]),
                            r=[("tok6", src)], w=[KT[wi // 2]], signal=(pr_ == 3))
                op("act", lambda e: e.activation(out=fT[:, 0:2, :, :], in_=pt[0][:].rearrange("p (w a) t -> p w a t", w=2), func=AF.Copy),
                   w=["fT01", KT[0]])
                nw = 2 if lat else 1
                op("dve", lambda e: e.tensor_copy(out=fT[:, 2:2 + nw, :, :], in_=pt[1][:, 0:4 * nw, :].rearrange("p (w a) t -> p w a t", w=nw)),
                   w=["fT23", KT[1]])
                nprod = 5 if lat else 3
                plist = [(1, 0, mX, True), (0, 1, mY, True), (2, 0, mX, False), (1, 3, mI, False), (2, 3, mI, False)][:nprod]
                rounds = [plist[0:3]] + ([plist[3:5]] if lat else [])
                pbase = 0
                for rnd in rounds:
                    for idx, (li, ri, mk, neg) in enumerate(rnd):
                        for q_ in range(4):
                            for par in range(2):
                                hb = par * 64
                                bank = idx * 2 + par
                                op("pe", lambda e, li=li, ri=ri, q_=q_, hb=hb, bank=bank: e.matmul(
                                    pb[bank][:, q_ * 128:(q_ + 1) * 128], lhsT=fT[hb:hb + 64, li, q_, :], rhs=fT[hb:hb + 64, ri, q_, :],
                                    start=True, stop=True), r=["fT01", "fT23"], w=[KB[bank]], signal=(q_ == 3))
                    for idx, (li, ri, mk, neg) in enumerate(rnd):
                        pi = pbase + idx
                        msk = (ntri if neg else tri_f)[:, mk, :].unsqueeze(1).to_broadcast([128, 4, 128])
                        for par in range(2):
                            bank = idx * 2 + par
                            dst = prod[:, pi].rearrange("p (q two) t -> p q two t", two=2)[:, :, par, :]
                            op("dve", lambda e, dst=dst, msk=msk, bank=bank: e.tensor_tensor(
                                out=dst, in0=pb[bank][:].rearrange("p (h t) -> p h t", t=128), in1=msk, op=ALU.mult),
                                r=["ntri", "tri_f"], w=[("prod", pi), KB[bank]])
                    pbase += len(rnd)
                if KRWL < 5:
                    continue
                Xf, Yf = prod[:, 0], prod[:, 1]
                h3 = lambda ap: ap.bitcast(BF16).rearrange("p (h t) -> p h t", t=128)
                A0, B0 = XY2[:, 0], XY2[:, 1]
                A1, B1, Tn = h3(akk[:]), h3(kw[:]), h3(kkn[:])
                idb8 = ident_b[:, :].unsqueeze(1).to_broadcast([128, 8, 128])
                bd = blkb[:, 0, :].unsqueeze(1).to_broadcast([128, 8, 128])
                dead = [("tok6", q_) for q_ in range(6)]
                def hk(k_, hf):
                    return (k_, "hf", hf)

                def mm8(bank0, lh, rh, rk, nm):
                    for h in range(8):
                        hf = h // 4
                        op("pe", lambda e, h=h: e.matmul(pb[bank0 + h // 4][:, (h % 4) * 128:(h % 4 + 1) * 128], lhsT=lh[:, h, :], rhs=rh[:, h, :],
                                                         start=True, stop=True), r=[hk(k_, hf) for k_ in rk], w=[KB[bank0 + hf]], signal=(h % 4 == 3))

                def ev8(bank0, dst, kd, extra=()):
                    for hf in range(2):
                        op("act", lambda e, hf=hf: e.activation(out=dst[:, hf * 4:hf * 4 + 4, :], in_=pb[bank0 + hf][:].rearrange("p (h t) -> p h t", t=128),
                                                                func=AF.Copy), r=list(extra), w=[hk(kd, hf), KB[bank0 + hf]])

                def acc8(bank0, dst, kd):
                    for hf in range(2):
                        op("dve", lambda e, hf=hf: e.tensor_tensor(out=dst[:, hf * 4:hf * 4 + 4, :], in0=pb[bank0 + hf][:].rearrange("p (h t) -> p h t", t=128),
                                                                   in1=dst[:, hf * 4:hf * 4 + 4, :], op=ALU.add), w=[hk(kd, hf), KB[bank0 + hf]])

                def msk8(dst, kd, src, ksrc, m_, extra=()):
                    for hf in range(2):
                        op("pool", lambda e, hf=hf: e.tensor_tensor(out=dst[:, hf * 4:hf * 4 + 4, :], in0=src[:, hf * 4:hf * 4 + 4, :],
                                                                    in1=m_.unsqueeze(1).to_broadcast([128, 4, 128]), op=op_), r=[ksrc, "blkb", "ident_b"] + list(extra),
                           w=[hk(kd, hf)])

                op_ = ALU.mult
                msk8(A0, "XY2a", Xf, ("prod", 0), blkb[:, 0, :])
                msk8(B0, "XY2b", Yf, ("prod", 1), blkb[:, 0, :])
                op_ = ALU.add
                for hf in range(2):
                    op("pool", lambda e, hf=hf: e.tensor_tensor(out=Tt[:, hf * 4:hf * 4 + 4, :], in0=A0[:, hf * 4:hf * 4 + 4, :],
                                                                in1=ident_b[:, :].unsqueeze(1).to_broadcast([128, 4, 128]), op=ALU.add),
                       r=[hk("XY2a", hf), "ident_b", "Tt"], w=[hk("Tt", hf)])
                    op("pool", lambda e, hf=hf: e.tensor_tensor(out=Tn[:, hf * 4:hf * 4 + 4, :], in0=B0[:, hf * 4:hf * 4 + 4, :],
                                                                in1=ident_b[:, :].unsqueeze(1).to_broadcast([128, 4, 128]), op=ALU.add),
                       r=[hk("XY2b", hf), "ident_b", "kkn"] + dead, w=[hk("Tn", hf)])
                op_ = ALU.mult
                Xc, Yc, kX, kY = A0, B0, "XY2a", "XY2b"
                for lvl in range(3):
                    Xn, Yn, kXn, kYn = (A1, B1, "A1", "B1") if lvl % 2 == 0 else (A0, B0, "XY2a", "XY2b")
                    mm8(2, Xc, Yc, [kX, kY], "Yn")
                    mm8(0, Yc, Xc, [kX, kY], "Xn")
                    ev8(2, Yn, kYn, dead + ["akk", "kw"])
                    ev8(0, Xn, kXn, dead + ["akk", "kw"])
                    mm8(4, Yn, Tt[:], [kYn, "Tt"], "XnTt")
                    mm8(0, Xn, Tn, [kXn, "Tn"], "YnTn")
                    acc8(4, Tt[:], "Tt")
                    acc8(0, Tn, "Tn")
                    Xc, Yc, kX, kY = Xn, Yn, kXn, kYn
                for mk in range(3):
                    lastm = mk == 2
                    msk8(A0, "XY2a", Xf, ("prod", 0), blkb[:, 1 + mk, :])
                    msk8(B0, "XY2b", Yf, ("prod", 1), blkb[:, 1 + mk, :])
                    mm8(0, B0, Tt[:], ["XY2b", "Tt"], "M1")
                    if not lastm:
                        mm8(2, A0, Tn, ["XY2a", "Tn"], "M2")
                    ev8(0, A1, "A1")
                    if not lastm:
                        ev8(2, B1, "B1")
                    mm8(4, Tn, A1, ["Tn", "A1"], "TtM1")
                    if not lastm:
                        mm8(0, Tt[:], B1, ["Tt", "B1"], "TnM2")
                    acc8(4, Tt[:], "Tt")
                    if not lastm:
                        acc8(0, Tn, "Tn")
                op("pool", lambda e: e.memset(st8[:, 24:25], 0.0),
                   r=[hk(k_, hf) for k_ in ("Tt", "Tn", "A1", "B1", "XY2a", "XY2b") for hf in range(2)], w=["Tt", "kkn", "akk", "kw", ("XY2", 0), ("XY2", 1)])
                if KRWL < 6:
                    continue
                for h in range(8):
                    op("pe", lambda e, h=h: e.matmul(pb[0][:, h * 64:(h + 1) * 64], lhsT=prod[:, 2, h, :], rhs=vb[:, h * 64:(h + 1) * 64], start=True, stop=True),
                       r=[("prod", 2), "vb"], w=[KB[0]], signal=(h == 7))
                op("act", lambda e: e.activation(out=Zs[:], in_=pb[0][:], func=AF.Copy), w=["Zs", KB[0]])
                for h in range(8):
                    op("pe", lambda e, h=h: e.matmul(pb[2][:, h * 64:(h + 1) * 64], lhsT=Tt[:, h, :], rhs=tok6[:, 0, h * 64:(h + 1) * 64], start=True, stop=True),
                       r=["Tt", ("tok6", 0)], w=[KB[2]], signal=(h == 7))
                op("act", lambda e: e.activation(out=Abs_[:], in_=pb[2][:], func=AF.Copy), w=["Abs", KB[2]])
                for h in range(8):
                    op("pe", lambda e, h=h: e.matmul(pb[1][:, h * 64:(h + 1) * 64], lhsT=Tt[:, h, :], rhs=Zs[:, h * 64:(h + 1) * 64], start=True, stop=True),
                       r=["Tt", "Zs"], w=[KB[1]], signal=(h == 7))
                op("act", lambda e: e.activation(out=nW[:], in_=pb[1][:], func=AF.Copy, scale=-1.0), w=["nW", KB[1]])
                for h in range(8):
                    pair, hb = h // 2, (h % 2) * 64
                    op("pe", lambda e, h=h, pair=pair, hb=hb: e.matmul(pb[4][hb:hb + 64, pair * 64:(pair + 1) * 64], lhsT=Abs_[:, h * 64:(h + 1) * 64],
                                                                       rhs=tok6[:, 5, h * 64:(h + 1) * 64], start=True, stop=True, tile_position=(0, hb)),
                       r=["Abs", ("tok6", 5)], w=[KB[4]], signal=(h == 7))
                for pair in range(4):
                    op("dve", lambda e, pair=pair: e.scalar_tensor_tensor(out=Gt[:, pair, :], in0=idp[:], scalar=PCf[:, pair:pair + 1],
                                                                          in1=pb[4][:, pair * 64:(pair + 1) * 64], op0=ALU.mult, op1=ALU.subtract),
                       r=["idp", "PCf"], w=["Gt", KB[4]], small=True)
                if KRWL < 7:
                    continue
                if lat:
                    for h in range(8):
                        pair, hb = h // 2, (h % 2) * 64
                        op("pe", lambda e, h=h, pair=pair, hb=hb: e.matmul(pb[3][hb:hb + 64, pair * 128:(pair + 1) * 128], lhsT=Abs_[:, h * 64:(h + 1) * 64],
                                                                           rhs=prod[:, 3, h, :], start=True, stop=True, tile_position=(0, hb)),
                           r=["Abs", ("prod", 3)], w=[KB[3]], signal=(h == 7))
                    op("dve", lambda e: e.tensor_tensor(out=RbT[:], in0=fT[:, 3, :, :], in1=pb[3][:].rearrange("p (a t) -> p a t", t=128), op=ALU.subtract),
                       r=["fT23"], w=["RbT", KB[3]])
                    for h in range(8):
                        pair, hb = h // 2, (h % 2) * 64
                        o_ = pb[5][:, h * 64:(h + 1) * 64]
                        op("pe", lambda e, h=h, o_=o_: e.matmul(o_, lhsT=prod[:, 4, h, :], rhs=vb[:, h * 64:(h + 1) * 64], start=True, stop=False),
                           r=[("prod", 4), "vb"], w=[KB[5]], signal=False)
                        op("pe", lambda e, h=h, o_=o_: e.matmul(o_, lhsT=prod[:, 3, h, :], rhs=nW[:, h * 64:(h + 1) * 64], start=False, stop=False),
                           r=[("prod", 3), "nW"], w=[KB[5]], signal=False)
                        op("pe", lambda e, h=h, o_=o_, pair=pair, hb=hb: e.matmul(o_, lhsT=RbT[hb:hb + 64, pair, :], rhs=H[hb:hb + 64, pair, :],
                                                                                 start=False, stop=True), r=["RbT", kH], w=[KB[5]], signal=(h == 7))
                    op("dve", lambda e: e.tensor_tensor(out=oacc[:, j, :], in0=pb[5][:], in1=oacc[:, j, :], op=ALU.add), w=["oacc", KB[5]])
                for h in range(8):
                    pair, hb = h // 2, (h % 2) * 64
                    o_ = pb[0][hb:hb + 64, pair * 64:(pair + 1) * 64]
                    op("pe", lambda e, h=h, o_=o_, hb=hb: e.matmul(o_, lhsT=tok6[:, 4, h * 64:(h + 1) * 64], rhs=vb[:, h * 64:(h + 1) * 64], start=True, stop=False,
                                                                   tile_position=(0, hb)), r=[("tok6", 4), "vb"], w=[KB[0]], signal=False)
                    op("pe", lambda e, h=h, o_=o_, hb=hb: e.matmul(o_, lhsT=tok6[:, 5, h * 64:(h + 1) * 64], rhs=nW[:, h * 64:(h + 1) * 64], start=False, stop=False,
                                                                   tile_position=(0, hb)), r=[("tok6", 5), "nW"], w=[KB[0]], signal=False)
                    op("pe", lambda e, h=h, o_=o_, pair=pair, hb=hb: e.matmul(o_, lhsT=Gt[hb:hb + 64, pair, :], rhs=H[hb:hb + 64, pair, :], start=False, stop=True,
                                                                             tile_position=(hb, hb)), r=["Gt", kH], w=[KB[0]], signal=(h == 7))
                op("act", lambda e, H=H: e.activation(out=H[:], in_=pb[0][:, 0:256].rearrange("p (a v) -> p a v", v=64), func=AF.Copy), w=[kH, KB[0]])
        if dbg and "oacc" in dbg and b == 0 and nvis >= NT:
            S.dma("sp", dbg_d["oacc"][:, :, :], oacc[:], r=["oacc"], chan="dbg")
        nfin = 16 if nvis >= NT else 0
        for j in range(nfin):
            o3 = oacc[:, j, :].rearrange("p (h f) -> p h f", f=64)
            op("dve", lambda e: e.tensor_reduce(out=st8[:, 0:8], in_=o3, axis=AX.X, op=ALU.add), r=["oacc"], w=["st8"], small=True)
            op("pool", lambda e: e.tensor_tensor(out=tA[:], in0=oacc[:, j, :], in1=oacc[:, j, :], op=ALU.mult), r=["oacc"], w=["tA"])
            op("dve", lambda e: e.tensor_reduce(out=st8[:, 8:16], in_=tA[:].rearrange("p (h f) -> p h f", f=64), axis=AX.X, op=ALU.add),
               r=["tA"], w=["st8b"], small=True)
            op("dve", lambda e: e.tensor_scalar(out=st8[:, 0:8], in0=st8[:, 0:8], scalar1=1.0 / 64, scalar2=None, op0=ALU.mult), w=["st8"], small=True)
            op("dve", lambda e: e.tensor_tensor(out=st8[:, 16:24], in0=st8[:, 0:8], in1=st8[:, 0:8], op=ALU.mult), r=["st8"], w=["st8c"], small=True)
            op("dve", lambda e: e.scalar_tensor_tensor(out=st8[:, 8:16], in0=st8[:, 8:16], scalar=1.0 / 64, in1=st8[:, 16:24], op0=ALU.mult, op1=ALU.subtract),
               r=["st8c"], w=["st8b"], small=True)
            op("dve", lambda e: e.tensor_scalar(out=st8[:, 8:16], in0=st8[:, 8:16], scalar1=64e-5, scalar2=None, op0=ALU.add), w=["st8b"], small=True)
            op("act", lambda e: e.activation(out=st8[:, 8:16], in_=st8[:, 8:16], func=AF.Sqrt), w=["st8b"], small=True)
            op("dve", lambda e: e.reciprocal(out=st8[:, 8:16], in_=st8[:, 8:16]), w=["st8b"], small=True)
            a3 = tA[:].rearrange("p (h f) -> p h f", f=64)
            op("dve", lambda e: e.tensor_tensor(out=a3, in0=o3, in1=st8[:, 0:8].unsqueeze(2).to_broadcast([128, 8, 64]), op=ALU.subtract),
               r=["oacc", "st8"], w=["tA"], small=True)
            op("dve", lambda e: e.tensor_tensor(out=a3, in0=a3, in1=st8[:, 8:16].unsqueeze(2).to_broadcast([128, 8, 64]), op=ALU.mult),
               r=["st8b"], w=["tA"], small=True)
            op("pool", lambda e: e.tensor_tensor(out=tA[:], in0=tA[:], in1=lngr[:], op=ALU.mult), r=["rwc"], w=["tA"])
            op("pool", lambda e: e.tensor_tensor(out=tA[:], in0=tA[:], in1=lnbr[:], op=ALU.add), r=["rwc"], w=["tA"])
            op("dve", lambda e: e.tensor_tensor(out=tA[:], in0=tA[:], in1=bon[:, j, :], op=ALU.add), r=["bon"], w=["tA"])
            op("pe", lambda e, j=j: e.transpose(out=pt[1][:, 0, :], in_=sgd[:, j, :], identity=ident_b[:]), r=["sgd"], w=[KT[1]])
            op("act", lambda e: e.activation(out=lT[:, 2, :], in_=pt[1][:, 0, :], func=AF.Copy), w=["lT", KT[1]])
            op("pe", lambda e: e.matmul(pb[2][:], lhsT=lT[:, 2, :], rhs=gup[:], start=True, stop=True), r=["lT", "rwc2"], w=[KB[2]])
            op("dve", lambda e: e.tensor_tensor(out=rwtok[:], in0=pb[2][:], in1=tA[:], op=ALU.mult), r=["tA"], w=["rwtok", KB[2]])
            for pr_ in range(4):
                op("pe", lambda e, pr_=pr_: e.transpose(out=pt[0][:, pr_, :], in_=rwtok[:, pr_ * 128:(pr_ + 1) * 128], identity=ident_b[:]),
                   r=["rwtok"], w=[KT[0]], signal=(pr_ == 3))
            op("act", lambda e, j=j: e.activation(out=mixT[:, 4:8, j * 128:(j + 1) * 128], in_=pt[0][:, 0:4, :], func=AF.Copy), w=["mixT_rw", KT[0]])
        if dbg and "rwT" in dbg and b == 0 and nvis >= NT:
            S.dma("sp", dbg_d["rwT"][:, :, :], mixT[:, 4:8, :], r=["mixT_rw"], chan="dbg")
        S.barrier()


def gate_row(S, sb, ps, st, G, b, m, name):
    modT, ident_f = G["modT"], G["ident_f"]
    row = sb(name, [128, D], stack=st)
    with contextlib.ExitStack() as s2:
        tmpb = sb("tmpb", [128, 128], stack=s2)
        ps_g = [ps("ps_g%d" % i, [128, 512], stack=s2) for i in range(2)]
        for j in range(8):
            S.op("dve", lambda e, j=j: e.tensor_copy(out=tmpb[:], in_=modT[:, m * 8 + j, b:b + 1].to_broadcast([128, 128])),
                 r=["modT"], w=["tmpb"])
            S.op("pe", lambda e, j=j: e.matmul(ps_g[j // 4][:, (j % 4) * 128:(j % 4 + 1) * 128], lhsT=tmpb[:], rhs=ident_f[:], start=True, stop=True),
                 r=["tmpb", "ident_f"], w=[("ps_g", j // 4)])
        for hf in range(2):
            S.op("dve", lambda e, hf=hf: e.tensor_copy(out=row[:, hf * 512:(hf + 1) * 512], in_=ps_g[hf][:]), w=[name, ("ps_g", hf)])
        S.barrier()
    return row


def stage_O(nc, S, sb, ps, b, G, dbg, dbg_d, mixT, hx2T, comb, rms_rstd):
    ident_f, epsc = G["ident_f"], G["epsc"]
    modT, gsT = G["modT"], G["gsT"]
    x_d, y_d = G["x_d"], G["y_d"]
    with contextlib.ExitStack() as st:
        g2row = gate_row(S, sb, ps, st, G, b, 2, "g2row")
        w_out = sb("w_out", [128, 8, D], BF16, stack=st)
        wcat = sb("wcat", [128, 8, 36], stack=st)
        bcat = sb("bcat", [128, 36], stack=st)
        xt = [sb("xto%d" % i, [128, D], stack=st) for i in range(2)]
        xm = sb("xm", [128, D], stack=st)
        junk = sb("junko", [128, D], BF16, stack=st)
        hf32 = sb("hf32", [128, 8, 128], stack=st)
        rstd = sb("rstdo", [128, 2], stack=st)
        rt = sb("rt", [128, 96], stack=st)
        ps_y = [ps("ps_y%d" % i, [128, 512], stack=st) for i in range(2)]
        ps_tr = [ps("ps_tr%d" % i, [128, 4, 128], stack=st) for i in range(2)]
        ps_l = ps("ps_lg", [128, 64], stack=st)
        S.dma("pool", w_out[:], G["wout_d"].rearrange("(kc p) n -> p kc n", p=128), w=["w_out"])
        S.dma("sp", wcat[:], G["wcat_d"].rearrange("(kc p) n -> p kc n", p=128), w=["wcat"])
        S.dma("sp", bcat[:], G["bcat_d"].partition_broadcast(128), w=["bcat"])
        for j in range(16):
            xb_ = xt[j % 2]
            kx = ("xto", j % 2)
            S.dma("sp", xb_[:], x_d[b, j * 128:(j + 1) * 128, :], w=[kx])
            for n in range(2):
                for kc in range(8):
                    S.op("pe", lambda e, n=n, kc=kc: e.matmul(ps_y[n][:], lhsT=mixT[:, kc, j * 128:(j + 1) * 128], rhs=w_out[:, kc, n * 512:(n + 1) * 512],
                                                              start=(kc == 0), stop=(kc == 7)), r=["mixT_na", "mixT_rw", "w_out"], w=[("ps_y", n)], signal=(kc == 7))
            for n in range(2):
                S.op("dve", lambda e, n=n: e.tensor_tensor(out=xm[:, n * 512:(n + 1) * 512], in0=ps_y[n][:], in1=g2row[:, n * 512:(n + 1) * 512], op=ALU.mult),
                     r=["g2row"], w=[("xm", n), ("ps_y", n)])
                S.op("pool", lambda e, n=n: e.tensor_tensor(out=xm[:, n * 512:(n + 1) * 512], in0=xm[:, n * 512:(n + 1) * 512], in1=xb_[:, n * 512:(n + 1) * 512], op=ALU.add),
                     r=[kx], w=[("xm", n)])
            S.dma("sp", y_d[b, j * 128:(j + 1) * 128, :], xm[:], r=[("xm", 0), ("xm", 1)], w=[("ymid", j)], chan=("yw", j % 2))
            rs = rstd[:, 0:1]
            S.op("act", lambda e: e.activation(out=junk[:], in_=xm[:], func=AF.Square, accum_out=rs), r=[("xm", 0), ("xm", 1)], w=["rstdo", "junko"])
            S.op("act", lambda e: e.activation(out=rs, in_=rs, func=AF.Sqrt, bias=epsc[:, 0:1], scale=1.0 / D), r=["epsc"], w=["rstdo"])
            S.op("dve", lambda e: e.reciprocal(out=rs, in_=rs), w=["rstdo"])
            S.op("dve", lambda e: e.tensor_scalar(out=xb_[:], in0=xm[:], scalar1=rs, scalar2=None, op0=ALU.mult), r=[("xm", 0), ("xm", 1), "rstdo"], w=[kx])
            for kc in range(8):
                S.op("pe", lambda e, kc=kc: e.transpose(out=ps_tr[kc // 4][:, kc % 4, :], in_=xb_[:, kc * 128:(kc + 1) * 128], identity=ident_f[:]),
                     r=[kx, "ident_f"], w=[("ps_tr", kc // 4)], signal=(kc % 4 == 3))
            for kc in range(8):
                S.op("dve" if kc < 4 else "act", (lambda e, kc=kc: e.tensor_scalar(out=hf32[:, kc, :], in0=ps_tr[kc // 4][:, kc % 4, :], scalar1=gsT[:, 1, kc, b:b + 1],
                                                                                     scalar2=modT[:, 24 + kc, b:b + 1], op0=ALU.mult, op1=ALU.add)) if kc < 4 else
                     (lambda e, kc=kc: e.activation(out=hf32[:, kc, :], in_=ps_tr[kc // 4][:, kc % 4, :], func=AF.Identity, bias=modT[:, 24 + kc, b:b + 1],
                                                    scale=gsT[:, 1, kc, b:b + 1])), r=["modT", "gsT"], w=[("hf32", kc), ("ps_tr", kc // 4)])
            S.op("pool", lambda e, j=j: e.tensor_copy(out=hx2T[:, :, j * 128:(j + 1) * 128], in_=hf32[:]), r=[("hf32", k_) for k_ in range(8)], w=["hx2T", "mixT_na", "mixT_rw"])
            for kc in range(8):
                S.op("pe", lambda e, kc=kc: e.matmul(ps_l[:, 0:36], lhsT=hf32[:, kc, :], rhs=wcat[:, kc, :], start=(kc == 0), stop=(kc == 7)),
                     r=[("hf32", kc), "wcat"], w=["ps_l"], signal=(kc == 7))
            lg, le = rt[:, 0:4], rt[:, 4:36]
            sm_ = lambda fn, r=(), w=("rt",), eng="dve": S.op(eng, fn, r=list(r), w=list(w), small=True)
            sm_(lambda e: e.tensor_tensor(out=rt[:, 0:36], in0=ps_l[:, 0:36], in1=bcat[:], op=ALU.add), r=["bcat"], w=["rt", "ps_l"])
            gmax, ngmax, se, pg = rt[:, 36:37], rt[:, 37:38], rt[:, 38:39], rt[:, 39:40]
            goh, esel = rt[:, 40:44], rt[:, 44:52]
            m1, m2, oh1, oh2 = rt[:, 52:53], rt[:, 53:54], rt[:, 54:62], rt[:, 62:70]
            e2, dd, w1, w2 = rt[:, 70:78], rt[:, 78:79], rt[:, 79:80], rt[:, 80:81]
            cw, tmp4 = rt[:, 81:89], rt[:, 89:93]
            sm_(lambda e: e.tensor_reduce(out=gmax, in_=lg, axis=AX.X, op=ALU.max))
            sm_(lambda e: e.tensor_scalar(out=ngmax, in0=gmax, scalar1=-1.0, scalar2=None, op0=ALU.mult))
            sm_(lambda e: e.tensor_scalar(out=goh, in0=lg, scalar1=gmax, scalar2=None, op0=ALU.is_equal))
            sm_(lambda e: e.activation(out=tmp4, in_=lg, func=AF.Exp, bias=ngmax, scale=1.0, accum_out=se), eng="act")
            sm_(lambda e: e.reciprocal(out=pg, in_=se))
            big = G["big32"]
            sm_(lambda e: e.tensor_tensor(out=big[:, 0:32].rearrange("p (g x) -> p g x", x=8), in0=le.rearrange("p (g x) -> p g x", x=8),
                                          in1=goh.unsqueeze(2).to_broadcast([128, 4, 8]), op=ALU.mult), w=["rt", "big32"])
            sm_(lambda e: e.tensor_reduce(out=esel, in_=big[:, 0:32].rearrange("p (g x) -> p x g", x=8), axis=AX.X, op=ALU.add), w=["rt", "big32"])
            sm_(lambda e: e.tensor_reduce(out=m1, in_=esel, axis=AX.X, op=ALU.max))
            sm_(lambda e: e.tensor_scalar(out=oh1, in0=esel, scalar1=m1, scalar2=None, op0=ALU.is_equal))
            sm_(lambda e: e.scalar_tensor_tensor(out=e2, in0=oh1, scalar=-1e30, in1=esel, op0=ALU.mult, op1=ALU.add))
            sm_(lambda e: e.tensor_reduce(out=m2, in_=e2, axis=AX.X, op=ALU.max))
            sm_(lambda e: e.tensor_scalar(out=oh2, in0=e2, scalar1=m2, scalar2=None, op0=ALU.is_equal))
            sm_(lambda e: e.tensor_tensor(out=dd, in0=m2, in1=m1, op=ALU.subtract))
            sm_(lambda e: e.activation(out=dd, in_=dd, func=AF.Exp), eng="act")
            sm_(lambda e: e.tensor_scalar(out=w1, in0=dd, scalar1=1.0, scalar2=None, op0=ALU.add))
            sm_(lambda e: e.reciprocal(out=w1, in_=w1))
            sm_(lambda e: e.tensor_tensor(out=w2, in0=dd, in1=w1, op=ALU.mult))
            sm_(lambda e: e.tensor_tensor(out=w1, in0=w1, in1=pg, op=ALU.mult))
            sm_(lambda e: e.tensor_tensor(out=w2, in0=w2, in1=pg, op=ALU.mult))
            sm_(lambda e: e.tensor_scalar(out=cw, in0=oh1, scalar1=w1, scalar2=None, op0=ALU.mult))
            sm_(lambda e: e.scalar_tensor_tensor(out=cw, in0=oh2, scalar=w2, in1=cw, op0=ALU.mult, op1=ALU.add))
            sm_(lambda e, j=j: e.tensor_tensor(out=comb[:, j, :].rearrange("p (g x) -> p g x", x=8), in0=goh.unsqueeze(2).to_broadcast([128, 4, 8]),
                                               in1=cw.unsqueeze(1).to_broadcast([128, 4, 8]), op=ALU.mult), w=["rt", "comb"])
        if dbg and "comb" in dbg and b == 0:
            S.dma("sp", dbg_d["comb"][:, :, :], comb[:], r=["comb"], chan="dbg")
        S.barrier()


def stage_MOE(nc, S, sb, ps, b, G, dbg, dbg_d, hx2T, comb):
    import os
    y_d = G["y_d"]
    with contextlib.ExitStack() as st:
        g5row = gate_row(S, sb, ps, st, G, b, 5, "g5row")
        acc = sb("acc", [128, 16, D], stack=st)
        w13 = [sb("w13_%d" % i, [128, 2, 8, 512], BF16, stack=st) for i in range(2)]
        w2s = [sb("w2s_%d" % i, [128, 4, D], BF16, stack=st) for i in range(2)]
        he = [sb("he%d" % i, [128, 4, 512], BF16, stack=st) for i in range(2)]
        sa = [sb("sa%d" % i, [128, 512], stack=st) for i in range(2)]
        xr = [sb("xr%d" % i, [128, D], stack=st) for i in range(2)]
        ps_a = [ps("ps_a%d" % i, [128, 512], stack=st) for i in range(2)]
        ps_b = [ps("ps_b%d" % i, [128, 512], stack=st) for i in range(2)]
        ps_o = [ps("ps_o%d" % i, [128, 512], stack=st) for i in range(4)]
        S.op("pool", lambda e: e.memset(acc[:], 0.0), w=[("acc", t_) for t_ in range(16)])
        nexp = int(os.environ.get("KEXP", 32))
        cnt = {"ab": 0, "o": 0}

        def emit_ab(ex, tg):
            wb = ex % 2
            if tg == 0:
                S.dma("pool", w13[wb][:, 0], G["w1_d"][ex].rearrange("(kc p) n -> p kc n", p=128), w=[("w1", wb)])
                S.dma("pool", w13[wb][:, 1], G["w3_d"][ex].rearrange("(kc p) n -> p kc n", p=128), w=[("w3", wb)])
                S.dma("pool", w2s[wb][:], G["w2_d"][ex].rearrange("(kc p) n -> p kc n", p=128), w=[("w2", wb)])
            hb_ = he[tg % 2]
            khe = ("he", tg % 2)
            for fc in range(4):
                ab = cnt["ab"] % 2
                cnt["ab"] += 1
                for which, pss, kname in ((0, ps_a, "ps_a"), (1, ps_b, "ps_b")):
                    for kc in range(8):
                        S.op("pe", lambda e, which=which, pss=pss, kc=kc, fc=fc, ab=ab: e.matmul(
                            pss[ab][:], lhsT=w13[wb][:, which, kc, fc * 128:(fc + 1) * 128], rhs=hx2T[:, kc, tg * 512:(tg + 1) * 512],
                            start=(kc == 0), stop=(kc == 7)), r=[("w1", wb), ("w3", wb), "hx2T"], w=[(kname, ab)], signal=(kc == 7))
                S.op("act", lambda e, ab=ab: e.activation(out=sa[ab][:], in_=ps_a[ab][:], func=AF.Silu), w=[("sa", ab), ("ps_a", ab)])
                S.op("dve", lambda e, ab=ab, fc=fc, hb_=hb_: e.tensor_tensor(out=hb_[:, fc, :], in0=ps_b[ab][:], in1=sa[ab][:], op=ALU.mult),
                     r=[("sa", ab)], w=[khe, ("ps_b", ab)])

        def emit_w2(ex, tg):
            wb = ex % 2
            hb_ = he[tg % 2]
            khe = ("he", tg % 2)
            for tt in range(4):
                tile = tg * 4 + tt
                for n in range(2):
                    ob = cnt["o"] % 4
                    cnt["o"] += 1
                    for fc in range(4):
                        S.op("pe", lambda e, fc=fc, n=n, ob=ob, tt=tt: e.matmul(
                            ps_o[ob][:], lhsT=hb_[:, fc, tt * 128:(tt + 1) * 128], rhs=w2s[wb][:, fc, n * 512:(n + 1) * 512],
                            start=(fc == 0), stop=(fc == 3)), r=[khe, ("w2", wb)], w=[("ps_o", ob)], signal=(fc == 3))
                    S.op("dve", lambda e, n=n, ob=ob, tile=tile: e.scalar_tensor_tensor(
                        out=acc[:, tile, n * 512:(n + 1) * 512], in0=ps_o[ob][:], scalar=comb[:, tile, ex:ex + 1],
                        in1=acc[:, tile, n * 512:(n + 1) * 512], op0=ALU.mult, op1=ALU.add), r=["comb"], w=[("acc", tile), ("ps_o", ob)])

        items = [(ex, tg) for ex in range(nexp) for tg in range(4)]
        emit_ab(*items[0])
        for k_ in range(len(items)):
            if k_ + 1 < len(items):
                emit_ab(*items[k_ + 1])
            emit_w2(*items[k_])
        for j in range(16):
            xb_ = xr[j % 2]
            kx = ("xr", j % 2)
            S.dma("sp", xb_[:], y_d[b, j * 128:(j + 1) * 128, :], r=[("ymid", j)], w=[kx])
            S.op("pool", lambda e, j=j: e.tensor_tensor(out=acc[:, j, :], in0=acc[:, j, :], in1=g5row[:], op=ALU.mult), r=["g5row"], w=[("acc", j)])
            S.op("pool", lambda e, j=j: e.tensor_tensor(out=xb_[:], in0=xb_[:], in1=acc[:, j, :], op=ALU.add), r=[("acc", j)], w=[kx])
            S.dma("sp", y_d[b, j * 128:(j + 1) * 128, :], xb_[:], r=[kx], w=[("yfin", j)], chan=("yw", j % 2))
        S.barrier()
```
